# Optimizing a Trainium2 kernel written in Bass

```python
import jax, jax.numpy as jnp
from jax import lax
import numpy as np

D_MODEL = 1024
BATCH = 32
SEQ = 2048
DEPTH = 4

GM_WIDTH = D_MODEL
GM_GROUPS = 8
GM_CHUNK = 128
LRU_WIDTH = D_MODEL
LRU_HEADS = 8
LRU_CONV = 4
LRU_C = 8.0
HG_HEADS = 8
HG_EXPAND = D_MODEL // HG_HEADS
HG_HEAD_DIM = D_MODEL // HG_HEADS
HG_KEY_WIDTH = HG_HEADS * HG_EXPAND
HG_WIDTH = HG_HEADS * HG_HEAD_DIM
HG_CHUNK = 32
N_BRANCH = 3
EPS = 1e-6
SPLIT_SIZES = (GM_WIDTH, GM_WIDTH, GM_WIDTH,
               LRU_WIDTH, LRU_WIDTH,
               HG_KEY_WIDTH, HG_KEY_WIDTH, HG_WIDTH, HG_WIDTH,
               N_BRANCH * D_MODEL)
IN_COLS = sum(SPLIT_SIZES)
SPLIT_POINTS = [int(s) for s in np.cumsum(SPLIT_SIZES)[:-1]]

kernel_name = "hybrid_gmlp_rglru_hgrn2_gated_merge"


def rms_norm(x, g):
    xf = x.astype(jnp.float32)
    y = xf * lax.rsqrt(jnp.mean(xf * xf, axis=-1, keepdims=True) + EPS)
    return (y * g.astype(jnp.float32)).astype(x.dtype)


def layer_norm(x, g, b):
    xf = x.astype(jnp.float32)
    mu = jnp.mean(xf, axis=-1, keepdims=True)
    xc = xf - mu
    y = xc * lax.rsqrt(jnp.mean(xc * xc, axis=-1, keepdims=True) + EPS)
    return (y * g.astype(jnp.float32) + b.astype(jnp.float32)).astype(x.dtype)


def chunked_spatial_gating(u, v, ln_g, ln_b, w_s, b_s):
    bsz, seq, width = v.shape
    v = layer_norm(v, ln_g, ln_b)
    vc = v.reshape(bsz, seq // GM_CHUNK, GM_CHUNK, GM_GROUPS, width // GM_GROUPS)
    causal = jnp.tril(jnp.ones((GM_CHUNK, GM_CHUNK), dtype=bool))
    w = jnp.where(causal[None], w_s, jnp.zeros_like(w_s))
    mixed = jnp.einsum('gts,bnsgc->bntgc', w, vc) + b_s.T[None, None, :, :, None]
    return u * mixed.reshape(bsz, seq, width)


def causal_depthwise_conv(x, w, b):
    k, width = w.shape
    out = lax.conv_general_dilated(
        x, w[:, None, :], window_strides=(1,), padding=[(k - 1, 0)],
        dimension_numbers=('NWC', 'WIO', 'NWC'), feature_group_count=width)
    return out + b


def block_diag_linear(x, w, b):
    bsz, seq, width = x.shape
    nh = w.shape[0]
    xh = x.reshape(bsz, seq, nh, width // nh)
    return jnp.einsum('bshi,hij->bshj', xh, w).reshape(bsz, seq, width) + b


def rg_lru(x, w_r, b_r, w_i, b_i, lam):
    f32 = jnp.float32
    r = jax.nn.sigmoid(block_diag_linear(x, w_r, b_r).astype(f32))
    i = jax.nn.sigmoid(block_diag_linear(x, w_i, b_i).astype(f32))
    log_a = -LRU_C * r * jax.nn.softplus(-lam.astype(f32))
    a = jnp.exp(log_a)
    mult = jnp.sqrt(-jnp.expm1(2.0 * log_a))
    bx = mult * (i * x.astype(f32))

    def step(h, ab):
        a_t, b_t = ab
        h = a_t * h + b_t
        return h, h

    h0 = jnp.zeros((x.shape[0], x.shape[2]), f32)
    _, hs = lax.scan(step, h0, (jnp.swapaxes(a, 0, 1), jnp.swapaxes(bx, 0, 1)))
    return jnp.swapaxes(hs, 0, 1).astype(x.dtype)


def hgrn2_chunked(q, f_raw, v, lower_bound):
    f32 = jnp.float32
    bsz, seq, nh, dk = q.shape
    dv = v.shape[-1]
    nc = seq // HG_CHUNK
    lb = lower_bound.reshape(nh, dk).astype(f32)
    fr = f_raw.astype(f32)
    f = lb + (1.0 - lb) * jax.nn.sigmoid(fr)
    k = (1.0 - lb) * jax.nn.sigmoid(-fr)
    g = jnp.log(f)

    def to_chunks(t):
        return jnp.moveaxis(t.reshape(bsz, nc, HG_CHUNK, nh, t.shape[-1]), 1, 0)

    gcum = jnp.cumsum(to_chunks(g), axis=2)
    causal = jnp.tril(jnp.ones((HG_CHUNK, HG_CHUNK), dtype=bool))[None, :, :, None, None]

    def step(state, inp):
        qc, kc, vc, gc = inp
        o_inter = jnp.einsum('bthk,bhkv->bthv', qc * jnp.exp(gc), state)
        diff = gc[:, :, None] - gc[:, None, :]
        decay = jnp.exp(jnp.where(causal, diff, -jnp.inf))
        scores = jnp.einsum('bthk,bshk,btshk->bhts', qc, kc, decay)
        o_intra = jnp.einsum('bhts,bshv->bthv', scores, vc)
        g_last = gc[:, -1]
        k_dec = kc * jnp.exp(g_last[:, None] - gc)
        state = state * jnp.exp(g_last)[..., None] + jnp.einsum('bshk,bshv->bhkv', k_dec, vc)
        return state, o_inter + o_intra

    s0 = jnp.zeros((bsz, nh, dk, dv), f32)
    _, out = lax.scan(step, s0, (to_chunks(q.astype(f32)), to_chunks(k),
                                 to_chunks(v.astype(f32)), gcum))
    return jnp.moveaxis(out, 0, 1).reshape(bsz, seq, nh, dv)


def hybrid_layer(x, pre_g, post_g, w_in, b_merge,
                 gm_ln_g, gm_ln_b, gm_w_s, gm_b_s,
                 lru_conv_w, lru_conv_b, lru_w_r, lru_b_r, lru_w_i, lru_b_i, lru_lambda,
                 hg_lb, hg_norm_g, w_a_proj, w_b_proj, w_c_proj, w_out):
    bsz, seq, _ = x.shape
    xn = rms_norm(x, pre_g)
    z = jnp.einsum('bsd,dn->bsn', xn, w_in)
    (gm_u, gm_v, gm_gate, lru_x, lru_gate,
     hg_q, hg_f, hg_i, hg_gate, merge) = jnp.split(z, SPLIT_POINTS, axis=-1)

    y_a = chunked_spatial_gating(jax.nn.gelu(gm_u), jax.nn.gelu(gm_v),
                                 gm_ln_g, gm_ln_b, gm_w_s, gm_b_s)
    y_a = jnp.einsum('bsw,wd->bsd', y_a * jax.nn.silu(gm_gate), w_a_proj)

    xb = causal_depthwise_conv(lru_x, lru_conv_w, lru_conv_b)
    y_b = rg_lru(xb, lru_w_r, lru_b_r, lru_w_i, lru_b_i, lru_lambda)
    y_b = jnp.einsum('bsw,wd->bsd', y_b * jax.nn.silu(lru_gate), w_b_proj)

    o = hgrn2_chunked(hg_q.reshape(bsz, seq, HG_HEADS, HG_EXPAND),
                      hg_f.reshape(bsz, seq, HG_HEADS, HG_EXPAND),
                      hg_i.reshape(bsz, seq, HG_HEADS, HG_HEAD_DIM), hg_lb)
    o = o * lax.rsqrt(jnp.mean(o * o, axis=-1, keepdims=True) + EPS)
    o = (o * hg_norm_g.astype(jnp.float32).reshape(HG_HEADS, HG_HEAD_DIM))
    o = o.reshape(bsz, seq, HG_WIDTH).astype(x.dtype)
    y_c = jnp.einsum('bsw,wd->bsd', o * jax.nn.silu(hg_gate), w_c_proj)

    gates = jax.nn.sigmoid(merge.reshape(bsz, seq, N_BRANCH, D_MODEL) + b_merge)
    merged = gates[:, :, 0] * y_a + gates[:, :, 1] * y_b + gates[:, :, 2] * y_c
    y = jnp.einsum('bsd,de->bse', merged, w_out)
    return x + rms_norm(y, post_g)


def setup_inputs(seed: int = 0) -> dict:
    key = jax.random.key(seed)
    ks = jax.random.split(key, 22)
    f32 = jnp.float32
    L, D = DEPTH, D_MODEL
    nrm = lambda k, shape, s: jax.random.normal(k, shape, f32) * s
    dh_lru = LRU_WIDTH // LRU_HEADS
    u = jax.random.uniform(ks[15], (L, LRU_WIDTH), f32, 0.9, 0.999)
    a_base = u ** (1.0 / LRU_C)
    return {
        "x": nrm(ks[0], (BATCH, SEQ, D), 1.0),
        "pre_norm_g": 1.0 + nrm(ks[1], (L, D), 0.02),
        "post_norm_g": 1.0 + nrm(ks[2], (L, D), 0.02),
        "w_in": nrm(ks[3], (L, D, IN_COLS), D ** -0.5),
        "b_merge": nrm(ks[4], (L, N_BRANCH, D), 0.02),
        "gm_ln_g": 1.0 + nrm(ks[5], (L, GM_WIDTH), 0.02),
        "gm_ln_b": nrm(ks[6], (L, GM_WIDTH), 0.02),
        "gm_w_s": nrm(ks[7], (L, GM_GROUPS, GM_CHUNK, GM_CHUNK), GM_CHUNK ** -0.5),
        "gm_b_s": 1.0 + nrm(ks[8], (L, GM_GROUPS, GM_CHUNK), 0.02),
        "lru_conv_w": nrm(ks[9], (L, LRU_CONV, LRU_WIDTH), LRU_CONV ** -0.5),
        "lru_conv_b": nrm(ks[10], (L, LRU_WIDTH), 0.02),
        "lru_w_r": nrm(ks[11], (L, LRU_HEADS, dh_lru, dh_lru), dh_lru ** -0.5),
        "lru_b_r": nrm(ks[12], (L, LRU_WIDTH), 0.02),
        "lru_w_i": nrm(ks[13], (L, LRU_HEADS, dh_lru, dh_lru), dh_lru ** -0.5),
        "lru_b_i": nrm(ks[14], (L, LRU_WIDTH), 0.02),
        "lru_lambda": jnp.log(a_base) - jnp.log1p(-a_base),
        "hg_lower_bounds": nrm(ks[16], (L, HG_KEY_WIDTH), 0.1),
        "hg_norm_g": 1.0 + nrm(ks[17], (L, HG_WIDTH), 0.02),
        "w_a_proj": nrm(ks[18], (L, GM_WIDTH, D), GM_WIDTH ** -0.5),
        "w_b_proj": nrm(ks[19], (L, LRU_WIDTH, D), LRU_WIDTH ** -0.5),
        "w_c_proj": nrm(ks[20], (L, HG_WIDTH, D), HG_WIDTH ** -0.5),
        "w_out": nrm(ks[21], (L, D, D), D ** -0.5),
    }


def reference(x, pre_norm_g, post_norm_g, w_in, b_merge, gm_ln_g, gm_ln_b, gm_w_s, gm_b_s,
              lru_conv_w, lru_conv_b, lru_w_r, lru_b_r, lru_w_i, lru_b_i, lru_lambda,
              hg_lower_bounds, hg_norm_g, w_a_proj, w_b_proj, w_c_proj, w_out):
    p = jax.nn.softmax(hg_lower_bounds.astype(jnp.float32), axis=0)
    lower_bounds = jnp.cumsum(p, axis=0) - p[0]
    h = x
    for l in range(DEPTH):
        h = hybrid_layer(h, pre_norm_g[l], post_norm_g[l], w_in[l], b_merge[l],
                         gm_ln_g[l], gm_ln_b[l], gm_w_s[l], gm_b_s[l],
                         lru_conv_w[l], lru_conv_b[l], lru_w_r[l], lru_b_r[l],
                         lru_w_i[l], lru_b_i[l], lru_lambda[l],
                         lower_bounds[l], hg_norm_g[l],
                         w_a_proj[l], w_b_proj[l], w_c_proj[l], w_out[l])
    return h
```

```python
import numpy as np
import concourse.bass as bass
import concourse.mybir as mybir
from concourse.bass_utils import run_bass_kernel_spmd

F32 = mybir.dt.float32
BF16 = mybir.dt.bfloat16
AF = mybir.ActivationFunctionType
ALU = mybir.AluOpType

D = 1024
KC = 8
T = 512
INC = 12288
NG = 34
GSZ = 4096
EPS = 1e-6
NPV = 17
NDV = 11
NSLOT = 4

CB_U, CB_V, CB_GA, CB_X, CB_GB, CB_Q, CB_F, CB_I, CB_GC, CB_M = 0, 8, 16, 24, 32, 40, 48, 56, 64, 72

GROUPS = [
    ("in", CB_V), ("in", CB_V + 4), ("auxA", 0),
    ("in", CB_U), ("in", CB_GA), ("in", CB_U + 4), ("in", CB_GA + 4),
    ("in", CB_M), ("a", 0), ("in", CB_M + 4), ("a", 4),
    ("auxB", 0), ("in", CB_X), ("in", CB_GB), ("in", CB_X + 4), ("in", CB_GB + 4),
    ("in", CB_M + 8), ("b", 0), ("in", CB_M + 12), ("b", 4),
    ("in", CB_I), ("in", CB_I + 4),
    ("in", CB_Q), ("in", CB_F), ("in", CB_GC), ("in", CB_Q + 4), ("in", CB_F + 4), ("in", CB_GC + 4),
    ("in", CB_M + 16), ("c", 0), ("in", CB_M + 20), ("c", 4),
    ("o", 0), ("o", 4),
]
assert len(GROUPS) == NG
GLEN = [1024 if k == 'auxA' else (2048 if k == 'auxB' else GSZ) for k, _ in GROUPS]


class Buf:
    __slots__ = ("w", "r", "excl")

    def __init__(self, excl=False):
        self.w = None
        self.r = {}
        self.excl = excl


class Eng:
    def __init__(self, name, h, sem, pe=False):
        self.name = name
        self.h = h
        self.sem = sem
        self.n = 0
        self.ninc = 0
        self.comp = {}
        self.seen = {}
        self.pe = pe


class Prog:
    def __init__(self, nc, need):
        self.nc = nc
        self.dry = need is None
        self.need = need
        self.waited = set()
        self.engs = {}
        self.dsems = {}

    def _waits(self, eng, reads, writes):
        deps = {}

        def add(d):
            if d is None:
                return
            k = d[0]
            if k not in deps or deps[k] < d[1]:
                deps[k] = d[1]
        for b in reads:
            add(b.w)
        for b in writes:
            add(b.w)
            for d in b.r.items():
                add(d)
        for k, val in deps.items():
            if eng.pe and k == eng.name:
                continue
            if eng.seen.get(k, 0) >= val:
                continue
            eng.seen[k] = val
            if self.dry:
                self.waited.add((k, val))
                continue
            if k in self.engs:
                src = self.engs[k]
                eng.h.wait_ge(src.sem, src.comp[val])
            else:
                eng.h.wait_ge(self.dsems[k], val)

    def _record(self, me, reads, writes):
        for b in reads:
            b.r[me[0]] = me[1]
        for b in writes:
            b.w = me
            b.r = {}

    def op(self, eng, fn, reads, writes):
        ex = [b for b in reads if b.excl]
        if ex:
            reads = [b for b in reads if not b.excl]
            writes = list(writes) + ex
        self._waits(eng, reads, writes)
        eng.n += 1
        if not self.dry:
            ins = fn(eng.h)
            if (eng.name, eng.n) in self.need:
                eng.ninc += 1
                eng.comp[eng.n] = eng.ninc
                ins.then_inc(eng.sem, 1)
        self._record((eng.name, eng.n), reads, writes)

    def dma(self, eng, semc, out, in_, reads, writes):
        self._waits(eng, reads, writes)
        semc[1] += 16
        if not self.dry:
            ins = eng.h.dma_start(out=out, in_=in_)
            ins.then_inc(semc[0], 16)
        self._record((semc[2], semc[1]), reads, writes)


DEBUG = {"branches": "abc", "stage": 9}


def build_program(L, NSEQ, NTILE, need=None):
    nc = bass.Bass("TRN2", target_bir_lowering=False)
    S = NTILE * T
    x_d = nc.dram_tensor("x", [NSEQ, S, D], F32, kind="ExternalInput").ap()
    ws_d = nc.dram_tensor("wstream", [L, NG, 128, GSZ], F32, kind="ExternalInput").ap()
    pv_d = nc.dram_tensor("pvec", [128, NPV * L * 8], F32, kind="ExternalInput").ap()
    bs_d = nc.dram_tensor("bsrow", [L, 1024], F32, kind="ExternalInput").ap()
    cst_d = nc.dram_tensor("cst", [128, 1280], F32, kind="ExternalInput").ap()
    out_d = nc.dram_tensor("out", [NSEQ, S, D], F32, kind="ExternalOutput").ap()
    wb_d = nc.dram_tensor("wbf", [L, NG, 128, GSZ], BF16, kind="Internal").ap()

    import contextlib
    es = contextlib.ExitStack()
    with es:
        def sb(name, shape, dt):
            return es.enter_context(nc.sbuf_tensor("sb_" + name, shape, dt))

        P = Prog(nc, need)

        def sem(name):
            return es.enter_context(nc.semaphore(name))

        def dsem(name):
            h = sem(name)
            P.dsems[name] = h
            return [h, 0, name]

        pe = Eng("pe", nc.tensor, sem("s_pe"), pe=True)
        act = Eng("act", nc.scalar, sem("s_act"))
        dve = Eng("dve", nc.vector, sem("s_dve"))
        pool = Eng("pool", nc.gpsimd, sem("s_pool"))
        sp = Eng("sp", nc.sync, sem("s_sp"))
        for e_ in (pe, act, dve, pool, sp):
            P.engs[e_.name] = e_

        xres = sb("xres", [128, KC, T], F32)
        xres_b = [Buf() for _ in range(KC)]
        merged = sb("merged", [128, KC, T], F32)
        merged_b = [Buf() for _ in range(KC)]
        hgst = sb("hgst", [128, L, 8 * 128], F32)
        hgst_b = [[Buf() for _ in range(8)] for _ in range(L)]
        stw = sb("stw", [128, 2, 4 * 128], F32)
        stw_b = [[Buf() for _ in range(4)] for _ in range(2)]
        lruh = sb("lruh", [128, L, 8], F32)
        lruh_b = [[Buf() for _ in range(8)] for _ in range(L)]
        xl = sb("xl", [128, KC, 3 + T], BF16)
        xl_b = [Buf() for _ in range(KC)]
        xtail = sb("xtail", [128, L, KC, 3], BF16)
        xtail_b = [Buf() for _ in range(L)]
        cst = sb("cst", [128, 1280], F32)
        cst_b = Buf()
        pv = sb("pv", [128, NPV, L, 8], F32)
        pv_b = Buf()
        dv = sb("dv", [128, NDV, L, 8], F32)
        dv_b = Buf()
        ptmp = sb("ptmp", [128, 6, L, 8], F32)
        ptmp_b = Buf()
        identb = sb("identb", [128, 128], BF16)
        ones = sb("ones", [128, 4, 128], BF16)
        constb = Buf()
        wring = sb("wring", [128, NSLOT, GSZ], BF16)
        wring_b = [Buf() for _ in range(NSLOT)]
        wsem = [dsem("w%d" % i) for i in range(NSLOT)]
        xn = sb("xn", [128, KC, T], BF16)
        xn_b = [Buf() for _ in range(KC)]
        NBIG = 4
        big = [sb("big%d" % i, [128, KC, T], BF16) for i in range(NBIG)]
        big_b = [[Buf() for _ in range(KC)] for _ in range(NBIG)]
        sall = sb("sall", [128, 16, 4 * 128], BF16)
        sall_b = [Buf() for _ in range(16)]
        NT32 = 8
        t32 = sb("t32", [128, NT32, T], F32)
        t32_b = [Buf() for _ in range(NT32)]
        NT16 = 6
        t16 = sb("t16", [128, NT16, T], BF16)
        t16_b = [Buf() for _ in range(NT16)]
        vtok = sb("vtok", [128, 1, 1024], F32)
        vtok_b = [Buf() for _ in range(1)]
        m2 = sb("m2", [128, 8, 128], F32)
        m2_b = Buf()
        wsm = sb("wsm", [128, 8, 128], BF16)
        wsm_b = Buf()
        wri = sb("wri", [128, 2048], BF16)
        wri_b = Buf()
        dg = sb("dg", [128, 4, 4, 128], BF16)
        dg_b = Buf()
        stat = sb("stat", [128, 4, 16], F32)
        stat_b = [Buf() for _ in range(4)]
        glast = sb("glast", [128, 4, 16], F32)
        glast_b = [Buf() for _ in range(4)]
        ps = es.enter_context(nc.psum_tensor("ps", [128, 8 * 512], F32))
        ps_b = [Buf(excl=True) for _ in range(8)]
        scr_b = [Buf() for _ in range(L)]
        cvsem = [dsem("cv%d" % l) for l in range(L)]
        iosem = dsem("io")
        misem = dsem("misc")
        pvsem = dsem("pvs")
        m2sem = dsem("m2s")

        ctr = {"ps": 0, "t32": 0, "t16": 0, "vt": 0}

        resv = set()

        def bank(reserve=False):
            while True:
                i = ctr["ps"] % 8
                ctr["ps"] += 1
                if i not in resv:
                    break
            if reserve:
                resv.add(i)
            return ps[:, i * 512:(i + 1) * 512], ps_b[i]

        def T32():
            i = ctr["t32"] % NT32
            ctr["t32"] += 1
            return t32[:, i, :], t32_b[i]

        def T16():
            i = ctr["t16"] % NT16
            ctr["t16"] += 1
            return t16[:, i, :], t16_b[i]

        for l in range(L):
            for g in range(NG):
                P.dma(pool, cvsem[l], wb_d[l, g, :, 0:GLEN[g]], ws_d[l, g, :, 0:GLEN[g]], [], [scr_b[l]])
        P.dma(sp, misem, cst[:], cst_d[:, :], [], [cst_b])
        P.dma(sp, pvsem, pv[:].rearrange("p a l c -> p (a l c)"), pv_d[:, :], [], [pv_b])
        P.op(dve, lambda e: e.tensor_copy(out=identb[:], in_=cst[:, 0:128]), [cst_b], [constb])
        for i, v in enumerate([1.0 / 1024, 1.0 / 4096, 1.0 / 128, 1.0]):
            P.op(dve, lambda e, i=i, v=v: e.memset(ones[:, i, :], v), [], [constb])
        P.op(dve, lambda e: e.memset(hgst[:].rearrange("p l n -> p (l n)"), 0.0), [], [b for bl in hgst_b for b in bl])
        P.op(dve, lambda e: e.memset(lruh[:].rearrange("p l n -> p (l n)"), 0.0), [], [b for bl in lruh_b for b in bl])
        P.op(dve, lambda e: e.memset(xtail[:].rearrange("p l c k -> p (l c k)"), 0.0), [], xtail_b)
        ident = cst[:, 0:128]
        mask_tril = cst[:, 128:256]
        mask_ch = cst[:, 256:768]
        rmask = cst[:, 768:1280]

        def pvs(i):
            return pv[:, i, :, :]

        def dvs(i):
            return dv[:, i, :, :]
        P.op(dve, lambda e: e.tensor_scalar(out=dvs(0), in0=pvs(1), scalar1=0.5, scalar2=None, op0=ALU.mult), [pv_b], [dv_b])
        for k in range(3):
            P.op(dve, lambda e, k=k: e.tensor_scalar(out=dvs(1 + k), in0=pvs(2 + k), scalar1=0.5, scalar2=None, op0=ALU.mult), [pv_b], [dv_b])
        P.op(dve, lambda e: e.tensor_scalar(out=dvs(4), in0=pvs(12), scalar1=0.5, scalar2=None, op0=ALU.mult), [pv_b], [dv_b])
        P.op(dve, lambda e: e.tensor_scalar(out=dvs(5), in0=pvs(13), scalar1=0.5, scalar2=None, op0=ALU.mult), [pv_b], [dv_b])
        P.op(act, lambda e: e.activation(out=ptmp[:, 0], in_=pvs(14), func=AF.Exp, scale=-1.0), [pv_b], [ptmp_b])
        P.op(act, lambda e: e.activation(out=ptmp[:, 1], in_=ptmp[:, 0], func=AF.Ln, bias=1.0, scale=1.0), [ptmp_b], [ptmp_b])
        P.op(dve, lambda e: e.tensor_scalar(out=dvs(6), in0=ptmp[:, 1], scalar1=-4.0, scalar2=None, op0=ALU.mult), [ptmp_b], [dv_b])
        P.op(dve, lambda e: e.tensor_scalar(out=dvs(7), in0=ptmp[:, 1], scalar1=-8.0, scalar2=None, op0=ALU.mult), [ptmp_b], [dv_b])
        lbx = pv[:, 15]
        mx = ptmp[:, 2, 0, :]
        P.op(dve, lambda e: e.tensor_copy(out=mx, in_=lbx[:, 0, :]), [pv_b], [ptmp_b])
        for l in range(1, L):
            P.op(dve, lambda e, l=l: e.tensor_tensor(out=mx, in0=mx, in1=lbx[:, l, :], op=ALU.max), [pv_b, ptmp_b], [ptmp_b])
        for l in range(L):
            P.op(dve, lambda e, l=l: e.tensor_tensor(out=ptmp[:, 3, l, :], in0=lbx[:, l, :], in1=mx, op=ALU.subtract), [pv_b, ptmp_b], [ptmp_b])
        P.op(act, lambda e: e.activation(out=ptmp[:, 4], in_=ptmp[:, 3], func=AF.Exp), [ptmp_b], [ptmp_b])
        sm = ptmp[:, 2, 1 % L if L > 1 else 0, :]
        sm = ptmp[:, 5, 0, :]
        P.op(dve, lambda e: e.tensor_copy(out=sm, in_=ptmp[:, 4, 0, :]), [ptmp_b], [ptmp_b])
        for l in range(1, L):
            P.op(dve, lambda e, l=l: e.tensor_tensor(out=sm, in0=sm, in1=ptmp[:, 4, l, :], op=ALU.add), [ptmp_b], [ptmp_b])
        if L > 1:
            rs = ptmp[:, 5, 1, :]
        else:
            rs = ptmp[:, 2, 0, :]
        P.op(dve, lambda e: e.reciprocal(out=rs, in_=sm), [ptmp_b], [ptmp_b])
        for l in range(L):
            P.op(dve, lambda e, l=l: e.tensor_tensor(out=ptmp[:, 3, l, :], in0=ptmp[:, 4, l, :], in1=rs, op=ALU.mult), [ptmp_b], [ptmp_b])
        P.op(dve, lambda e: e.memset(ptmp[:, 4, 0, :], 0.0), [ptmp_b], [ptmp_b])
        for l in range(1, L):
            P.op(dve, lambda e, l=l: e.tensor_tensor(out=ptmp[:, 4, l, :], in0=ptmp[:, 4, l - 1, :], in1=ptmp[:, 3, l, :], op=ALU.add), [ptmp_b], [ptmp_b])
        P.op(dve, lambda e: e.tensor_scalar(out=dvs(8), in0=ptmp[:, 4], scalar1=-0.5, scalar2=0.5, op0=ALU.mult, op1=ALU.add), [ptmp_b], [dv_b])
        P.op(dve, lambda e: e.tensor_scalar(out=dvs(9), in0=ptmp[:, 4], scalar1=0.5, scalar2=0.5, op0=ALU.mult, op1=ALU.add), [ptmp_b], [dv_b])
        P.op(dve, lambda e: e.tensor_scalar(out=dvs(10), in0=ptmp[:, 4], scalar1=0.5, scalar2=-0.5, op0=ALU.mult, op1=ALU.add), [ptmp_b], [dv_b])

        stream = {"issued": 0, "consumed": 0, "released": 0}
        units = [(b, j, l) for b in range(NSEQ) for j in range(NTILE) for l in range(L)]
        total_groups = len(units) * NG

        def issue_loads(upto):
            while stream["issued"] < min(upto, total_groups):
                k = stream["issued"]
                u, g = divmod(k, NG)
                l = units[u][2]
                s = k % NSLOT
                P.dma(sp, wsem[s], wring[:, s, 0:GLEN[g]], wb_d[l, g, :, 0:GLEN[g]], [scr_b[l]], [wring_b[s]])
                stream["issued"] += 1

        def next_group():
            k = stream["consumed"]
            issue_loads(stream["released"] + NSLOT)
            assert k < stream["issued"], (k, stream)
            stream["consumed"] += 1
            s = k % NSLOT
            return wring[:, s, :].rearrange("p (b k c) -> p b k c", b=4, k=8), wring_b[s], wring[:, s, :]

        def release(n=1):
            stream["released"] += n
            assert stream["released"] <= stream["consumed"]
            issue_loads(stream["released"] + NSLOT)

        def mm_fm(w, wb, blk, rhs_ap, rhs_b, n=T, reserve=False):
            pa, pb = bank(reserve)
            for kc in range(KC):
                P.op(pe, lambda e, kc=kc: e.matmul(pa[:, 0:n], lhsT=w[:, blk, kc, :], rhs=rhs_ap(kc),
                                                   start=(kc == 0), stop=(kc == KC - 1)),
                     [wb, rhs_b[kc]], [pb])
            return pa, pb

        def mm_tm(w, wb, tb, lhs, lhs_b):
            pa, pb = bank()
            for kc in range(KC):
                P.op(pe, lambda e, kc=kc: e.matmul(pa, lhsT=lhs[:, kc, tb * 128:(tb + 1) * 128], rhs=w[:, :, kc, :],
                                                   start=(kc == 0), stop=(kc == KC - 1)),
                     [wb, lhs_b[kc]], [pb])
            return pa, pb

        def rms_rstd(src_ap, src_bufs, ones_idx, nchunks, from_psum=False):
            pa, pb = bank()
            for c in range(nchunks):
                sq, sqb = T16()
                P.op(act, lambda e, c=c, sq=sq: e.activation(out=sq, in_=src_ap(c), func=AF.Square), [src_bufs[c]], [sqb])
                P.op(pe, lambda e, c=c, sq=sq: e.matmul(pa, lhsT=ones[:, ones_idx, :], rhs=sq, start=(c == 0), stop=(c == nchunks - 1)),
                     [sqb, constb], [pb])
            sd, sdb = T32()
            if DEBUG["stage"] == 0.2:
                return sd, sdb
            P.op(act, lambda e: e.activation(out=sd, in_=pa, func=AF.Sqrt, bias=EPS, scale=1.0), [pb], [sdb])
            rstd, rb = T32()
            if DEBUG["stage"] == 0.4:
                return sd, sdb
            P.op(dve, lambda e: e.reciprocal(out=rstd, in_=sd), [sdb], [rb])
            return rstd, rb

        def gate_merge(l, br, ypa, ypb, mpa, mpb, d):
            th, thb = T32()
            P.op(act, lambda e: e.activation(out=th, in_=mpa, func=AF.Tanh, bias=dv[:, 1 + br, l, d:d + 1], scale=0.5), [mpb, dv_b], [thb])
            if DEBUG["stage"] == 1.7:
                return
            if br == DEBUG["branches"].find("abc"[br]) == 0 or "abc"[br] == DEBUG["branches"][0]:
                P.op(dve, lambda e: e.scalar_tensor_tensor(out=merged[:, d, :], in0=th, scalar=1.0, in1=ypa, op0=ALU.add, op1=ALU.mult),
                     [thb, ypb], [merged_b[d]])
            else:
                tm, tmb = T32()
                P.op(dve, lambda e: e.scalar_tensor_tensor(out=tm, in0=th, scalar=1.0, in1=ypa, op0=ALU.add, op1=ALU.mult),
                     [thb, ypb], [tmb])
                P.op(pool, lambda e: e.tensor_tensor(out=merged[:, d, :], in0=merged[:, d, :], in1=tm, op=ALU.add),
                     [tmb, merged_b[d]], [merged_b[d]])

        def proj_and_merge(l, br, yin, yin_b):
            for half in range(2):
                wm, wmb, _ = next_group()
                wp, wpb, _ = next_group()
                for i in range(4):
                    d = half * 4 + i
                    mpa, mpb = mm_fm(wm, wmb, i, lambda kc: xn[:, kc, :], xn_b)
                    ypa, ypb = mm_fm(wp, wpb, i, lambda kc: yin[:, kc, :], yin_b)
                    if "abc"[br] in DEBUG["branches"] and DEBUG["stage"] != 1.6:
                        gate_merge(l, br, ypa, ypb, mpa, mpb, d)
                release(2)

        def unit(l):
            rstd, rb = rms_rstd(lambda c: xres[:, c, :], xres_b, 0, KC)
            if DEBUG["stage"] < 0.8:
                return
            for c in range(KC):
                P.op(dve, lambda e, c=c: e.scalar_tensor_tensor(out=xn[:, c, :], in0=xres[:, c, :], scalar=pv[:, 0, l, c:c + 1], in1=rstd,
                                                                  op0=ALU.mult, op1=ALU.mult),
                     [xres_b[c], rb, pv_b], [xn_b[c]])

            if DEBUG["stage"] < 1:
                return
            nbf, nbf_b = big[0], big_b[0]
            nview = nbf[:].rearrange("p a t -> p (a t)").rearrange("p (tb n) -> p tb n", tb=4)
            wv = [next_group(), next_group()]
            for tb in range(4):
                vi = 0
                ctr["vt"] += 1
                for half in range(2):
                    pa, pb = mm_tm(wv[half][0], wv[half][1], tb, xn, xn_b)
                    P.op(act, lambda e, pa=pa, half=half, vi=vi: e.activation(out=vtok[:, vi, half * 512:(half + 1) * 512], in_=pa, func=AF.Gelu_apprx_tanh),
                         [pb], [vtok_b[vi]])
                if DEBUG["stage"] == 1.1:
                    continue
                st6 = stat[:, tb, 0:12]
                for half in range(2):
                    P.op(dve, lambda e, half=half, vi=vi: e.bn_stats(out=stat[:, tb, half * 6:(half + 1) * 6], in_=vtok[:, vi, half * 512:(half + 1) * 512]),
                         [vtok_b[vi]], [stat_b[tb]])
                P.op(dve, lambda e: e.bn_aggr(out=stat[:, tb, 12:14], in_=st6), [stat_b[tb]], [stat_b[tb]])
                P.op(act, lambda e: e.activation(out=stat[:, tb, 14:15], in_=stat[:, tb, 13:14], func=AF.Sqrt, bias=EPS, scale=1.0), [stat_b[tb]], [stat_b[tb]])
                P.op(dve, lambda e: e.reciprocal(out=stat[:, tb, 15:16], in_=stat[:, tb, 14:15]), [stat_b[tb]], [stat_b[tb]])
                P.op(dve, lambda e, vi=vi: e.tensor_scalar(out=nview[:, tb, :], in0=vtok[:, vi, :], scalar1=stat[:, tb, 12:13], scalar2=stat[:, tb, 15:16],
                                                           op0=ALU.subtract, op1=ALU.mult),
                     [vtok_b[vi], stat_b[tb]], nbf_b)
            release(2)
            if DEBUG["stage"] in (1.1, 1.2):
                return
            _, auxb, auxflat = next_group()
            wsT = auxflat[:, 0:1024].rearrange("p (g t) -> p g t", g=8)
            P.op(pool, lambda e: e.tensor_tensor(out=wsm[:], in0=wsT, in1=mask_tril.unsqueeze(1).to_broadcast([128, 8, 128]), op=ALU.mult),
                 [auxb, cst_b], [wsm_b])
            release(1)
            if DEBUG["stage"] == 1.3:
                return
            P.dma(sp, m2sem, m2[:].rearrange("p g t -> p (g t)"), bs_d[l:l + 1, :].partition_broadcast(128), [], [m2_b])
            for hf in range(2):
                pa, pb = bank()
                P.op(pe, lambda e, pa=pa, hf=hf: e.matmul(pa, lhsT=ones[:, 3, :], rhs=wsm[:, hf * 4:(hf + 1) * 4, :], start=True, stop=True),
                     [wsm_b, constb], [pb])
                for gg in range(4):
                    g = hf * 4 + gg
                    P.op(dve, lambda e, pa=pa, gg=gg, g=g: e.scalar_tensor_tensor(out=m2[:, g, :], in0=pa[:, gg * 128:(gg + 1) * 128], scalar=pv[:, 6, l, g:g + 1],
                                                                                  in1=m2[:, g, :], op0=ALU.mult, op1=ALU.add),
                         [pb, m2_b, pv_b], [m2_b])
            if DEBUG["stage"] == 1.4:
                return
            yin, yin_b = big[1], big_b[1]
            for half in range(2):
                wu, wub, _ = next_group()
                wg, wgb, _ = next_group()
                for i in range(4):
                    c = half * 4 + i
                    upa, upb = mm_fm(wu, wub, i, lambda kc: xn[:, kc, :], xn_b)
                    ug, ugb = T16()
                    P.op(act, lambda e: e.activation(out=ug, in_=upa, func=AF.Gelu_apprx_tanh), [upb], [ugb])
                    gpa, gpb = mm_fm(wg, wgb, i, lambda kc: xn[:, kc, :], xn_b)
                    sg, sgb = T16()
                    P.op(act, lambda e: e.activation(out=sg, in_=gpa, func=AF.Silu), [gpb], [sgb])
                    spa, spb = bank()
                    for tb in range(4):
                        P.op(pe, lambda e, tb=tb: e.matmul(spa[:, tb * 128:(tb + 1) * 128], lhsT=nview[:, tb, c * 128:(c + 1) * 128], rhs=wsm[:, c, :],
                                                           start=True, stop=True),
                             nbf_b + [wsm_b], [spb])
                    mx_, mxb = T32()
                    P.op(dve, lambda e: e.scalar_tensor_tensor(out=mx_.rearrange("p (a t) -> p a t", a=4), in0=spa.rearrange("p (a t) -> p a t", a=4),
                                                               scalar=pv[:, 5, l, c:c + 1],
                                                               in1=m2[:, c, :].unsqueeze(1).to_broadcast([128, 4, 128]), op0=ALU.mult, op1=ALU.add),
                         [spb, m2_b, pv_b], [mxb])
                    P.op(pool, lambda e: e.tensor_tensor(out=ug, in0=ug, in1=sg, op=ALU.mult), [ugb, sgb], [ugb])
                    P.op(pool, lambda e: e.tensor_tensor(out=yin[:, c, :], in0=mx_, in1=ug, op=ALU.mult), [mxb, ugb], [yin_b[c]])
                release(2)
            if DEBUG["stage"] == 1.5:
                return
            proj_and_merge(l, 0, yin, yin_b)

            if DEBUG["stage"] < 2:
                return
            _, auxb, auxflat = next_group()
            P.op(pool, lambda e: e.tensor_copy(out=wri[:], in_=auxflat[:, 0:2048]), [auxb], [wri_b])
            release(1)
            auxb = wri_b
            wr = wri[:, 0:1024].rearrange("p (h j) -> p h j", h=8)
            wi = wri[:, 1024:2048].rearrange("p (h j) -> p h j", h=8)
            P.op(pool, lambda e: e.tensor_copy(out=xl[:, :, 0:3], in_=xtail[:, l]), [xtail_b[l]], xl_b)
            yin, yin_b = big[2], big_b[2]
            for half in range(2):
                wx, wxb, _ = next_group()
                wg, wgb, _ = next_group()
                for k in range(4):
                    P.op(pool, lambda e, k=k: e.tensor_tensor(out=dg[:, k], in0=ident.unsqueeze(1).to_broadcast([128, 4, 128]),
                                                               in1=pv[:, 7 + k, l, half * 4:half * 4 + 4].unsqueeze(2).to_broadcast([128, 4, 128]), op=ALU.mult),
                         [cst_b, pv_b], [dg_b])
                for pair in range(2):
                    ii = [pair * 2, pair * 2 + 1]
                    cc_ = [half * 4 + i for i in ii]
                    st_ = {}
                    for i, c in zip(ii, cc_):
                        xpa, xpb = mm_fm(wx, wxb, i, lambda kc: xn[:, kc, :], xn_b)
                        P.op(act, lambda e: e.activation(out=xl[:, c, 3:3 + T], in_=xpa, func=AF.Copy), [xpb], [xl_b[c]])
                    for i, c in zip(ii, cc_):
                        cpa, cpb = bank()
                        for k in range(4):
                            P.op(pe, lambda e, k=k: e.matmul(cpa, lhsT=dg[:, k, i, :], rhs=xl[:, c, k:k + T], start=(k == 0), stop=(k == 3)),
                                 [dg_b, xl_b[c]], [cpb])
                        xbf, xbfb = T32()
                        P.op(act, lambda e: e.activation(out=xbf, in_=cpa, func=AF.Identity, bias=pv[:, 11, l, c:c + 1], scale=1.0), [cpb, pv_b], [xbfb])
                        xbb, xbbb = T16()
                        P.op(dve, lambda e: e.tensor_scalar(out=xbb, in0=cpa, scalar1=pv[:, 11, l, c:c + 1], scalar2=None, op0=ALU.add), [cpb, pv_b], [xbbb])
                        st_[c] = {"xbf": (xbf, xbfb), "xbb": (xbb, xbbb)}
                    for i, c in zip(ii, cc_):
                        xbb, xbbb = st_[c]["xbb"]
                        rpa, rpb = bank()
                        P.op(pe, lambda e: e.matmul(rpa, lhsT=wr[:, c, :], rhs=xbb, start=True, stop=True), [auxb, xbbb], [rpb])
                        ipa, ipb = bank()
                        P.op(pe, lambda e: e.matmul(ipa, lhsT=wi[:, c, :], rhs=xbb, start=True, stop=True), [auxb, xbbb], [ipb])
                        st_[c]["r"] = (rpa, rpb)
                        st_[c]["i"] = (ipa, ipb)
                    for i, c in zip(ii, cc_):
                        rpa, rpb = st_[c]["r"]
                        ipa, ipb = st_[c]["i"]
                        thr, thrb = T32()
                        P.op(act, lambda e: e.activation(out=thr, in_=rpa, func=AF.Tanh, bias=dv[:, 4, l, c:c + 1], scale=0.5), [rpb, dv_b], [thrb])
                        thi, thib = T32()
                        P.op(act, lambda e: e.activation(out=thi, in_=ipa, func=AF.Tanh, bias=dv[:, 5, l, c:c + 1], scale=0.5), [ipb, dv_b], [thib])
                        st_[c]["thr"] = (thr, thrb)
                        st_[c]["thi"] = (thi, thib)
                    for i, c in zip(ii, cc_):
                        st_[c]["g"] = mm_fm(wg, wgb, i, lambda kc: xn[:, kc, :], xn_b)
                    for i, c in zip(ii, cc_):
                        thr, thrb = st_[c]["thr"]
                        a2, a2b = T32()
                        P.op(act, lambda e: e.activation(out=a2, in_=thr, func=AF.Exp, bias=dv[:, 7, l, c:c + 1], scale=dv[:, 7, l, c:c + 1]), [thrb, dv_b], [a2b])
                        P.op(act, lambda e: e.activation(out=thr, in_=thr, func=AF.Exp, bias=dv[:, 6, l, c:c + 1], scale=dv[:, 6, l, c:c + 1]), [thrb, dv_b], [thrb])
                        st_[c]["a2"] = (a2, a2b)
                    for i, c in zip(ii, cc_):
                        a2, a2b = st_[c]["a2"]
                        P.op(act, lambda e: e.activation(out=a2, in_=a2, func=AF.Sqrt, bias=0.25, scale=-0.25), [a2b], [a2b])
                    for i, c in zip(ii, cc_):
                        gpa, gpb = st_[c]["g"]
                        sg, sgb = T16()
                        P.op(act, lambda e: e.activation(out=sg, in_=gpa, func=AF.Silu), [gpb], [sgb])
                        st_[c]["sg"] = (sg, sgb)
                    for i, c in zip(ii, cc_):
                        xbf, xbfb = st_[c]["xbf"]
                        thi, thib = st_[c]["thi"]
                        aa, aab = st_[c]["thr"]
                        a2, a2b = st_[c]["a2"]
                        sg, sgb = st_[c]["sg"]
                        P.op(dve, lambda e: e.scalar_tensor_tensor(out=thi, in0=thi, scalar=1.0, in1=xbf, op0=ALU.add, op1=ALU.mult), [thib, xbfb], [thib])
                        P.op(pool, lambda e: e.tensor_tensor(out=thi, in0=thi, in1=a2, op=ALU.mult), [thib, a2b], [thib])
                        P.op(dve, lambda e: e.tensor_tensor_scan(out=xbf, data0=aa, data1=thi, initial=lruh[:, l, c:c + 1], op0=ALU.mult, op1=ALU.add),
                             [aab, thib, lruh_b[l][c]], [xbfb])
                        P.op(pool, lambda e: e.tensor_copy(out=lruh[:, l, c:c + 1], in_=xbf[:, T - 1:T]), [xbfb], [lruh_b[l][c]])
                        P.op(pool, lambda e: e.tensor_tensor(out=yin[:, c, :], in0=xbf, in1=sg, op=ALU.mult), [xbfb, sgb], [yin_b[c]])
                release(2)
            P.op(pool, lambda e: e.tensor_copy(out=xtail[:, l], in_=xl[:, :, T:T + 3]), xl_b, [xtail_b[l]])
            proj_and_merge(l, 1, yin, yin_b)

            if DEBUG["stage"] < 3:
                return
            vt_, vt_b = big[0], big_b[0]
            vview = vt_[:].rearrange("p a t -> p (a t)").rearrange("p (tb n) -> p tb n", tb=4)
            wv = [next_group(), next_group()]
            for tb in range(4):
                for half in range(2):
                    pa, pb = mm_tm(wv[half][0], wv[half][1], tb, xn, xn_b)
                    P.op(act, lambda e, pa=pa, half=half, tb=tb: e.activation(out=vview[:, tb, half * 512:(half + 1) * 512], in_=pa, func=AF.Copy),
                         [pb], vt_b)
            release(2)
            yin, yin_b = big[1], big_b[1]
            qg_, qg_b = big[2], big_b[2]
            kd_, kd_b = big[3], big_b[3]
            for hg in range(2):
                wq, wqb, _ = next_group()
                wf, wfb, _ = next_group()
                for pair in range(2):
                    ii = [pair * 2, pair * 2 + 1]
                    st_ = {}
                    for i in ii:
                        fpa, fpb = mm_fm(wf, wfb, i, lambda kc: xn[:, kc, :], xn_b)
                        th, thb = T32()
                        P.op(act, lambda e: e.activation(out=th, in_=fpa, func=AF.Tanh, scale=0.5), [fpb], [thb])
                        st_[i] = {"th": (th, thb)}
                    for i in ii:
                        st_[i]["q"] = mm_fm(wq, wqb, i, lambda kc: xn[:, kc, :], xn_b)
                    for i in ii:
                        h = hg * 4 + i
                        th, thb = st_[i]["th"]
                        ff, ffb = T32()
                        P.op(dve, lambda e: e.tensor_scalar(out=ff, in0=th, scalar1=dv[:, 8, l, h:h + 1], scalar2=dv[:, 9, l, h:h + 1], op0=ALU.mult, op1=ALU.add),
                             [thb, dv_b], [ffb])
                        G, Gb = T32()
                        P.op(dve, lambda e: e.tensor_tensor_scan(out=G, data0=rmask, data1=ff, initial=1.0, op0=ALU.max, op1=ALU.mult), [ffb, cst_b], [Gb])
                        P.op(pool, lambda e: e.tensor_copy(out=glast[:, i, :], in_=G.rearrange("p (c t) -> p c t", t=32)[:, :, 31]), [Gb], [glast_b[i]])
                        rG = ff
                        P.op(dve, lambda e: e.reciprocal(out=rG, in_=G), [Gb, ffb], [ffb])
                        P.op(pool, lambda e: e.tensor_scalar(out=th, in0=th, scalar1=dv[:, 10, l, h:h + 1], scalar2=dv[:, 8, l, h:h + 1], op0=ALU.mult, op1=ALU.add),
                             [thb, dv_b], [thb])
                        kg, kgb = T16()
                        P.op(pool, lambda e: e.tensor_tensor(out=kg, in0=th, in1=rG, op=ALU.mult), [thb, ffb], [kgb])
                        kdec, kdecb = T16()
                        P.op(pool, lambda e: e.tensor_tensor(out=kdec.rearrange("p (c t) -> p c t", t=32), in0=kg.rearrange("p (c t) -> p c t", t=32),
                                                             in1=glast[:, i, :].unsqueeze(2).to_broadcast([128, 16, 32]), op=ALU.mult),
                             [kgb, glast_b[i]], [kdecb])
                        qpa, qpb = st_[i]["q"]
                        P.op(dve, lambda e: e.tensor_tensor(out=qg_[:, i, :], in0=qpa, in1=G, op=ALU.mult), [qpb, Gb], [qg_b[i]])
                        st_[i]["kg"] = (kg, kgb)
                        st_[i]["kdec"] = (kdec, kdecb)
                    for i in ii:
                        kg, kgb = st_[i]["kg"]
                        kdec, kdecb = st_[i]["kdec"]
                        spa, spb = bank()
                        for tb in range(4):
                            P.op(pe, lambda e, tb=tb: e.matmul(spa[:, tb * 128:(tb + 1) * 128], lhsT=kg[:, tb * 128:(tb + 1) * 128],
                                                               rhs=qg_[:, i, tb * 128:(tb + 1) * 128], start=True, stop=True),
                                 [kgb, qg_b[i]], [spb])
                        P.op(dve, lambda e: e.tensor_tensor(out=kd_[:, 4 + i, :], in0=spa, in1=mask_ch, op=ALU.mult), [spb, cst_b], [kd_b[4 + i]])
                        tpa, tpb = bank()
                        tpbf = tpa.bitcast(BF16)
                        for tb in range(4):
                            P.op(pe, lambda e, tb=tb: e.transpose(tpbf[:, tb * 128:(tb + 1) * 128], kdec[:, tb * 128:(tb + 1) * 128], identb[:]),
                                 [kdecb, constb], [tpb])
                        P.op(act, lambda e: e.activation(out=kd_[:, i, :], in_=tpbf[:, 0:512], func=AF.Copy), [tpb], [kd_b[i]])
                release(2)
                wgc, wgcb, _ = next_group()
                for i in range(4):
                    h = hg * 4 + i
                    P.op(act, lambda e, i=i, h=h: e.activation(out=sall[:, 0, i * 128:(i + 1) * 128], in_=hgst[:, l, h * 128:(h + 1) * 128], func=AF.Copy),
                         [hgst_b[l][h]], [sall_b[0]])
                sgs = {}
                for c in range(16):
                    tb, cc = divmod(c, 4)
                    upa, upb = bank()
                    for i in range(4):
                        h = hg * 4 + i
                        P.op(pe, lambda e, i=i, h=h: e.matmul(upa[:, i * 128:(i + 1) * 128], lhsT=kd_[cc * 32:(cc + 1) * 32, i, tb * 128:(tb + 1) * 128],
                                                             rhs=vview[cc * 32:(cc + 1) * 32, tb, h * 128:(h + 1) * 128], start=True, stop=True,
                                                             tile_position=(cc * 32, 0)),
                             [kd_b[i]] + vt_b, [upb])
                    for i in range(4):
                        h = hg * 4 + i
                        if c == 0:
                            src, srcb = hgst[:, l, h * 128:(h + 1) * 128], hgst_b[l][h]
                        else:
                            src, srcb = stw[:, c % 2, i * 128:(i + 1) * 128], stw_b[c % 2][i]
                        if c == 15:
                            dst, dstb = hgst[:, l, h * 128:(h + 1) * 128], hgst_b[l][h]
                        else:
                            dst, dstb = stw[:, (c + 1) % 2, i * 128:(i + 1) * 128], stw_b[(c + 1) % 2][i]
                        P.op(dve, lambda e, i=i, src=src, dst=dst: e.scalar_tensor_tensor(out=dst, in0=src, scalar=glast[:, i, c:c + 1], in1=upa[:, i * 128:(i + 1) * 128],
                                                                                         op0=ALU.mult, op1=ALU.add),
                             [srcb, glast_b[i], upb], [dstb])
                        if c < 15:
                            P.op(act, lambda e, i=i, dst=dst: e.activation(out=sall[:, c + 1, i * 128:(i + 1) * 128], in_=dst, func=AF.Copy),
                                 [dstb], [sall_b[c + 1]])
                    if c % 4 == 3:
                        i = c // 4
                        sgs[i] = mm_fm(wgc, wgcb, i, lambda kc: xn[:, kc, :], xn_b, reserve=True)
                release(1)
                for i in range(4):
                    h = hg * 4 + i
                    opa, opb = bank()
                    for tb in range(4):
                        P.op(pe, lambda e, tb=tb, h=h, i=i: e.matmul(opa[:, tb * 128:(tb + 1) * 128], lhsT=vview[:, tb, h * 128:(h + 1) * 128],
                                                                     rhs=kd_[:, 4 + i, tb * 128:(tb + 1) * 128], start=True, stop=False),
                             vt_b + [kd_b[4 + i]], [opb])
                        for cc in range(4):
                            c = tb * 4 + cc
                            P.op(pe, lambda e, c=c, i=i, cc=cc: e.matmul(opa[:, c * 32:(c + 1) * 32], lhsT=sall[:, c, i * 128:(i + 1) * 128],
                                                                         rhs=qg_[:, i, c * 32:(c + 1) * 32], start=False, stop=(cc == 3)),
                                 [sall_b[c], qg_b[i]], [opb])
                    rstd, rb = rms_rstd(lambda c_: opa, [opb], 2, 1)
                    on, onb = T32()
                    P.op(dve, lambda e: e.scalar_tensor_tensor(out=on, in0=opa, scalar=pv[:, 16, l, h:h + 1], in1=rstd, op0=ALU.mult, op1=ALU.mult),
                         [opb, rb, pv_b], [onb])
                    gpa, gpb = sgs[i]
                    sg, sgb = T16()
                    P.op(act, lambda e: e.activation(out=sg, in_=gpa, func=AF.Silu), [gpb], [sgb])
                    resv.discard(ps_b.index(gpb))
                    P.op(pool, lambda e: e.tensor_tensor(out=yin[:, h, :], in0=on, in1=sg, op=ALU.mult), [onb, sgb], [yin_b[h]])
            proj_and_merge(l, 2, yin, yin_b)

            if DEBUG["stage"] < 4:
                return
            mbf, mbf_b = big[2], big_b[2]
            for c in range(KC):
                P.op(act, lambda e, c=c: e.activation(out=mbf[:, c, :], in_=merged[:, c, :], func=AF.Copy), [merged_b[c]], [mbf_b[c]])
            ppa, ppb = bank(reserve=True)
            for half in range(2):
                wo, wob, _ = next_group()
                for i in range(4):
                    d = half * 4 + i
                    ypa, ypb = mm_fm(wo, wob, i, lambda kc: mbf[:, kc, :], mbf_b)
                    sq, sqb = T16()
                    P.op(act, lambda e: e.activation(out=sq, in_=ypa, func=AF.Square), [ypb], [sqb])
                    P.op(pe, lambda e, d=d, sq=sq: e.matmul(ppa, lhsT=ones[:, 1, :], rhs=sq, start=(d == 0), stop=(d == 7)), [sqb, constb], [ppb])
                    P.op(dve, lambda e, d=d: e.tensor_scalar(out=merged[:, d, :], in0=ypa, scalar1=dv[:, 0, l, d:d + 1], scalar2=None, op0=ALU.mult),
                         [ypb, dv_b], [merged_b[d]])
                release(1)
            resv.clear()
            sd, sdb = T32()
            P.op(act, lambda e: e.activation(out=sd, in_=ppa, func=AF.Sqrt, bias=EPS, scale=1.0), [ppb], [sdb])
            rstd, rb = T32()
            P.op(dve, lambda e: e.reciprocal(out=rstd, in_=sd), [sdb], [rb])
            for d in range(KC):
                P.op(dve, lambda e, d=d: e.tensor_tensor(out=merged[:, d, :], in0=merged[:, d, :], in1=rstd, op=ALU.mult), [merged_b[d], rb], [merged_b[d]])
                P.op(pool, lambda e, d=d: e.tensor_tensor(out=xres[:, d, :], in0=xres[:, d, :], in1=merged[:, d, :], op=ALU.add),
                     [merged_b[d], xres_b[d]], [xres_b[d]])

        xio = merged[:].rearrange("p a t -> p (a t)").rearrange("p (tb n) -> p tb n", tb=4)
        for b in range(NSEQ):
            if b > 0:
                P.op(dve, lambda e: e.memset(hgst[:].rearrange("p l n -> p (l n)"), 0.0), [], [bb for bl in hgst_b for bb in bl])
                P.op(dve, lambda e: e.memset(lruh[:].rearrange("p l n -> p (l n)"), 0.0), [], [bb for bl in lruh_b for bb in bl])
                P.op(dve, lambda e: e.memset(xtail[:].rearrange("p l c k -> p (l c k)"), 0.0), [], xtail_b)
            for j in range(NTILE):
                P.dma(pool, iosem, xio, x_d[b, j * T:(j + 1) * T, :].rearrange("(tb p) n -> p tb n", p=128), [], merged_b)
                for c in range(KC):
                    pa, pb = bank()
                    for tb in range(4):
                        P.op(pe, lambda e, tb=tb, c=c: e.transpose(pa[:, tb * 128:(tb + 1) * 128], xio[:, tb, c * 128:(c + 1) * 128], ident),
                             merged_b + [cst_b], [pb])
                    P.op(act, lambda e, c=c, pa=pa: e.activation(out=xres[:, c, :], in_=pa, func=AF.Copy), [pb], [xres_b[c]])
                for l in range(L):
                    unit(l)
                for tb in range(4):
                    for hf in range(2):
                        pa, pb = bank()
                        for cc in range(4):
                            c = hf * 4 + cc
                            P.op(pe, lambda e, cc=cc, c=c, tb=tb: e.transpose(pa[:, cc * 128:(cc + 1) * 128], xres[:, c, tb * 128:(tb + 1) * 128], ident),
                                 [xres_b[c], cst_b], [pb])
                        P.op(act, lambda e, pa=pa, tb=tb, hf=hf: e.activation(out=xio[:, tb, hf * 512:(hf + 1) * 512], in_=pa, func=AF.Copy), [pb], merged_b)
                P.dma(pool, iosem, out_d[b, j * T:(j + 1) * T, :].rearrange("(tb p) n -> p tb n", p=128), xio, merged_b, [])
        if P.dry:
            return P.waited
        nc.gpsimd.wait_ge(iosem[0], iosem[1])
        stats = {e.name: (e.n, e.ninc) for e in (pe, act, dve, pool)}
        print("instr counts (ops, sem incs)", stats, "groups", stream)
    return nc


def _colblock(w, b0):
    sub = w[:, b0 * 128:(b0 + 4) * 128]
    return sub.reshape(8, 128, 4, 128).transpose(1, 2, 0, 3)


def make_consts():
    cst = np.zeros((128, 1280), np.float32)
    cst[:, 0:128] = np.eye(128, dtype=np.float32)
    s = np.arange(128)[:, None]
    t = np.arange(128)[None, :]
    cst[:, 128:256] = (s <= t).astype(np.float32)
    mch = ((s // 32 == t // 32) & (s <= t)).astype(np.float32)
    cst[:, 256:768] = np.tile(mch, (1, 4))
    rm = np.zeros(512, np.float32)
    rm[::32] = 1.0
    cst[:, 768:1280] = rm[None, :]
    return cst


def prep_shared(inp, L):
    ws = np.empty((L, NG, 128, GSZ), np.float32)
    proj = {"a": inp["w_a_proj"], "b": inp["w_b_proj"], "c": inp["w_c_proj"], "o": inp["w_out"]}
    for l in range(L):
        for g, (kind, b0) in enumerate(GROUPS):
            if kind == "in":
                ws[l, g] = _colblock(inp["w_in"][l], b0).reshape(128, GSZ)
            elif kind == "auxA":
                ws[l, g] = 0.0
                ws[l, g, :, 0:1024] = inp["gm_w_s"][l].transpose(2, 0, 1).reshape(128, 1024)
            elif kind == "auxB":
                ws[l, g] = 0.0
                ws[l, g, :, 0:1024] = inp["lru_w_r"][l].transpose(1, 0, 2).reshape(128, 1024)
                ws[l, g, :, 1024:2048] = inp["lru_w_i"][l].transpose(1, 0, 2).reshape(128, 1024)
            else:
                ws[l, g] = _colblock(proj[kind][l], b0).reshape(128, GSZ)
    names = [("pre_norm_g", None), ("post_norm_g", None), ("b_merge", 0), ("b_merge", 1), ("b_merge", 2),
             ("gm_ln_g", None), ("gm_ln_b", None), ("lru_conv_w", 0), ("lru_conv_w", 1), ("lru_conv_w", 2), ("lru_conv_w", 3),
             ("lru_conv_b", None), ("lru_b_r", None), ("lru_b_i", None), ("lru_lambda", None), ("hg_lower_bounds", None), ("hg_norm_g", None)]
    pvec = np.empty((128, NPV, L, 8), np.float32)
    for i, (n, k) in enumerate(names):
        a = inp[n][:L] if k is None else inp[n][:L, k]
        pvec[:, i] = a.reshape(L, 8, 128).transpose(2, 0, 1)
    bsrow = np.ascontiguousarray(inp["gm_b_s"][:L].reshape(L, 1024))
    return {"wstream": ws, "pvec": pvec.reshape(128, -1), "bsrow": bsrow, "cst": make_consts()}


def run(inputs, L, n_cores, nseq_per_core, ntile):
    inp = {k: np.asarray(v) for k, v in inputs.items()}
    shared = prep_shared(inp, L)
    need = build_program(L, nseq_per_core, ntile, None)
    nc = build_program(L, nseq_per_core, ntile, need)
    x = np.ascontiguousarray(inp["x"], dtype=np.float32)
    in_maps = []
    for c in range(n_cores):
        m = dict(shared)
        m["x"] = np.ascontiguousarray(x[c * nseq_per_core:(c + 1) * nseq_per_core])
        in_maps.append(m)
    res = run_bass_kernel_spmd(nc, in_maps, core_ids=list(range(n_cores)))
    return np.concatenate([r["out"] for r in res.results], axis=0)


def kernel(**inputs):
    return run(inputs, 4, 8, 4, 4)
```

```python
import numpy as np
import concourse.bass as bass
import concourse.mybir as mybir
from concourse.bass_utils import run_bass_kernel_spmd

F32 = mybir.dt.float32
BF16 = mybir.dt.bfloat16
AF = mybir.ActivationFunctionType
ALU = mybir.AluOpType

D = 1024
KC = 8
T = 512
INC = 12288
NG = 34
GSZ = 4096
EPS = 1e-6
NPV = 17
NDV = 11
NSLOT = 4

CB_U, CB_V, CB_GA, CB_X, CB_GB, CB_Q, CB_F, CB_I, CB_GC, CB_M = 0, 8, 16, 24, 32, 40, 48, 56, 64, 72

GROUPS = [
    ("in", CB_V), ("in", CB_V + 4), ("auxA", 0),
    ("in", CB_U), ("in", CB_GA), ("in", CB_U + 4), ("in", CB_GA + 4),
    ("in", CB_M), ("a", 0), ("in", CB_M + 4), ("a", 4),
    ("auxB", 0), ("in", CB_X), ("in", CB_GB), ("in", CB_X + 4), ("in", CB_GB + 4),
    ("in", CB_M + 8), ("b", 0), ("in", CB_M + 12), ("b", 4),
    ("in", CB_I), ("in", CB_I + 4),
    ("in", CB_Q), ("in", CB_F), ("in", CB_GC), ("in", CB_Q + 4), ("in", CB_F + 4), ("in", CB_GC + 4),
    ("in", CB_M + 16), ("c", 0), ("in", CB_M + 20), ("c", 4),
    ("o", 0), ("o", 4),
]
assert len(GROUPS) == NG
GLEN = [1024 if k == 'auxA' else (2048 if k == 'auxB' else GSZ) for k, _ in GROUPS]


class Buf:
    __slots__ = ("w", "r", "excl")

    def __init__(self, excl=False):
        self.w = None
        self.r = {}
        self.excl = excl


class Eng:
    def __init__(self, name, h, sem, pe=False):
        self.name = name
        self.h = h
        self.sem = sem
        self.n = 0
        self.ninc = 0
        self.comp = {}
        self.seen = {}
        self.pe = pe


class Prog:
    def __init__(self, nc, need):
        self.nc = nc
        self.dry = need is None
        self.need = need
        self.waited = set()
        self.engs = {}
        self.dsems = {}

    def _waits(self, eng, reads, writes):
        deps = {}

        def add(d):
            if d is None:
                return
            k = d[0]
            if k not in deps or deps[k] < d[1]:
                deps[k] = d[1]
        for b in reads:
            add(b.w)
        for b in writes:
            add(b.w)
            for d in b.r.items():
                add(d)
        for k, val in deps.items():
            if eng.pe and k == eng.name:
                continue
            if eng.seen.get(k, 0) >= val:
                continue
            eng.seen[k] = val
            if self.dry:
                self.waited.add((k, val))
                continue
            if k in self.engs:
                src = self.engs[k]
                eng.h.wait_ge(src.sem, src.comp[val])
            else:
                eng.h.wait_ge(self.dsems[k], val)

    def _record(self, me, reads, writes):
        for b in reads:
            b.r[me[0]] = me[1]
        for b in writes:
            b.w = me
            b.r = {}

    def op(self, eng, fn, reads, writes):
        ex = [b for b in reads if b.excl]
        if ex:
            reads = [b for b in reads if not b.excl]
            writes = list(writes) + ex
        self._waits(eng, reads, writes)
        eng.n += 1
        if not self.dry:
            ins = fn(eng.h)
            if (eng.name, eng.n) in self.need:
                eng.ninc += 1
                eng.comp[eng.n] = eng.ninc
                ins.then_inc(eng.sem, 1)
        self._record((eng.name, eng.n), reads, writes)

    def dma(self, eng, semc, out, in_, reads, writes):
        self._waits(eng, reads, writes)
        semc[1] += 16
        if not self.dry:
            ins = eng.h.dma_start(out=out, in_=in_)
            ins.then_inc(semc[0], 16)
        self._record((semc[2], semc[1]), reads, writes)


DEBUG = {"branches": "abc", "stage": 9}


def build_program(L, NSEQ, NTILE, need=None):
    nc = bass.Bass("TRN2", target_bir_lowering=False)
    S = NTILE * T
    x_d = nc.dram_tensor("x", [NSEQ, S, D], F32, kind="ExternalInput").ap()
    ws_d = nc.dram_tensor("wstream", [L, NG, 128, GSZ], F32, kind="ExternalInput").ap()
    pv_d = nc.dram_tensor("pvec", [128, NPV * L * 8], F32, kind="ExternalInput").ap()
    bs_d = nc.dram_tensor("bsrow", [L, 1024], F32, kind="ExternalInput").ap()
    cst_d = nc.dram_tensor("cst", [128, 1280], F32, kind="ExternalInput").ap()
    out_d = nc.dram_tensor("out", [NSEQ, S, D], F32, kind="ExternalOutput").ap()
    wb_d = nc.dram_tensor("wbf", [L, NG, 128, GSZ], BF16, kind="Internal").ap()

    import contextlib
    es = contextlib.ExitStack()
    with es:
        def sb(name, shape, dt):
            return es.enter_context(nc.sbuf_tensor("sb_" + name, shape, dt))

        P = Prog(nc, need)

        def sem(name):
            return es.enter_context(nc.semaphore(name))

        def dsem(name):
            h = sem(name)
            P.dsems[name] = h
            return [h, 0, name]

        pe = Eng("pe", nc.tensor, sem("s_pe"), pe=True)
        act = Eng("act", nc.scalar, sem("s_act"))
        dve = Eng("dve", nc.vector, sem("s_dve"))
        pool = Eng("pool", nc.gpsimd, sem("s_pool"))
        sp = Eng("sp", nc.sync, sem("s_sp"))
        for e_ in (pe, act, dve, pool, sp):
            P.engs[e_.name] = e_

        xres = sb("xres", [128, KC, T], F32)
        xres_b = [Buf() for _ in range(KC)]
        merged = sb("merged", [128, KC, T], F32)
        merged_b = [Buf() for _ in range(KC)]
        hgst = sb("hgst", [128, L, 8 * 128], F32)
        hgst_b = [[Buf() for _ in range(8)] for _ in range(L)]
        stw = sb("stw", [128, 2, 4 * 128], F32)
        stw_b = [[Buf() for _ in range(4)] for _ in range(2)]
        lruh = sb("lruh", [128, L, 8], F32)
        lruh_b = [[Buf() for _ in range(8)] for _ in range(L)]
        xl = sb("xl", [128, KC, 3 + T], BF16)
        xl_b = [Buf() for _ in range(KC)]
        xtail = sb("xtail", [128, L, KC, 3], BF16)
        xtail_b = [Buf() for _ in range(L)]
        cst = sb("cst", [128, 1280], F32)
        cst_b = Buf()
        pv = sb("pv", [128, NPV, L, 8], F32)
        pv_b = Buf()
        dv = sb("dv", [128, NDV, L, 8], F32)
        dv_b = Buf()
        ptmp = sb("ptmp", [128, 6, L, 8], F32)
        ptmp_b = Buf()
        identb = sb("identb", [128, 128], BF16)
        ones = sb("ones", [128, 4, 128], BF16)
        constb = Buf()
        wring = sb("wring", [128, NSLOT, GSZ], BF16)
        wring_b = [Buf() for _ in range(NSLOT)]
        wsem = [dsem("w%d" % i) for i in range(NSLOT)]
        xn = sb("xn", [128, KC, T], BF16)
        xn_b = [Buf() for _ in range(KC)]
        NBIG = 4
        big = [sb("big%d" % i, [128, KC, T], BF16) for i in range(NBIG)]
        big_b = [[Buf() for _ in range(KC)] for _ in range(NBIG)]
        sall = sb("sall", [128, 16, 4 * 128], BF16)
        sall_b = [Buf() for _ in range(16)]
        NT32 = 8
        t32 = sb("t32", [128, NT32, T], F32)
        t32_b = [Buf() for _ in range(NT32)]
        NT16 = 6
        t16 = sb("t16", [128, NT16, T], BF16)
        t16_b = [Buf() for _ in range(NT16)]
        vtok = sb("vtok", [128, 1, 1024], F32)
        vtok_b = [Buf() for _ in range(1)]
        m2 = sb("m2", [128, 8, 128], F32)
        m2_b = Buf()
        wsm = sb("wsm", [128, 8, 128], BF16)
        wsm_b = Buf()
        wri = sb("wri", [128, 2048], BF16)
        wri_b = Buf()
        dg = sb("dg", [128, 4, 4, 128], BF16)
        dg_b = Buf()
        stat = sb("stat", [128, 4, 16], F32)
        stat_b = [Buf() for _ in range(4)]
        glast = sb("glast", [128, 4, 16], F32)
        glast_b = [Buf() for _ in range(4)]
        ps = es.enter_context(nc.psum_tensor("ps", [128, 8 * 512], F32))
        ps_b = [Buf(excl=True) for _ in range(8)]
        scr_b = [[Buf() for _ in range(NG)] if l == 0 else [Buf()] * NG for l in range(L)]
        cvsem = [[dsem("cv0_%d" % g) for g in range(NG)] if l == 0 else [dsem("cv%d" % l)] * NG for l in range(L)]
        iosem = dsem("io")
        misem = dsem("misc")
        pvsem = dsem("pvs")
        m2sem = dsem("m2s")

        ctr = {"ps": 0, "t32": 0, "t16": 0, "vt": 0}

        resv = set()

        def bank(reserve=False):
            while True:
                i = ctr["ps"] % 8
                ctr["ps"] += 1
                if i not in resv:
                    break
            if reserve:
                resv.add(i)
            return ps[:, i * 512:(i + 1) * 512], ps_b[i]

        def T32():
            i = ctr["t32"] % NT32
            ctr["t32"] += 1
            return t32[:, i, :], t32_b[i]

        def T16():
            i = ctr["t16"] % NT16
            ctr["t16"] += 1
            return t16[:, i, :], t16_b[i]

        for l in range(L):
            for g in range(NG):
                P.dma(pool, cvsem[l][g], wb_d[l, g, :, 0:GLEN[g]], ws_d[l, g, :, 0:GLEN[g]], [], [scr_b[l][g]])
        P.dma(sp, misem, cst[:], cst_d[:, :], [], [cst_b])
        P.dma(sp, pvsem, pv[:].rearrange("p a l c -> p (a l c)"), pv_d[:, :], [], [pv_b])
        P.op(dve, lambda e: e.tensor_copy(out=identb[:], in_=cst[:, 0:128]), [cst_b], [constb])
        for i, v in enumerate([1.0 / 1024, 1.0 / 4096, 1.0 / 128, 1.0]):
            P.op(dve, lambda e, i=i, v=v: e.memset(ones[:, i, :], v), [], [constb])
        P.op(dve, lambda e: e.memset(hgst[:].rearrange("p l n -> p (l n)"), 0.0), [], [b for bl in hgst_b for b in bl])
        P.op(dve, lambda e: e.memset(lruh[:].rearrange("p l n -> p (l n)"), 0.0), [], [b for bl in lruh_b for b in bl])
        P.op(dve, lambda e: e.memset(xtail[:].rearrange("p l c k -> p (l c k)"), 0.0), [], xtail_b)
        ident = cst[:, 0:128]
        mask_tril = cst[:, 128:256]
        mask_ch = cst[:, 256:768]
        rmask = cst[:, 768:1280]

        def pvs(i):
            return pv[:, i, :, :]

        def dvs(i):
            return dv[:, i, :, :]
        P.op(dve, lambda e: e.tensor_scalar(out=dvs(0), in0=pvs(1), scalar1=0.5, scalar2=None, op0=ALU.mult), [pv_b], [dv_b])
        for k in range(3):
            P.op(dve, lambda e, k=k: e.tensor_scalar(out=dvs(1 + k), in0=pvs(2 + k), scalar1=0.5, scalar2=None, op0=ALU.mult), [pv_b], [dv_b])
        P.op(dve, lambda e: e.tensor_scalar(out=dvs(4), in0=pvs(12), scalar1=0.5, scalar2=None, op0=ALU.mult), [pv_b], [dv_b])
        P.op(dve, lambda e: e.tensor_scalar(out=dvs(5), in0=pvs(13), scalar1=0.5, scalar2=None, op0=ALU.mult), [pv_b], [dv_b])
        P.op(act, lambda e: e.activation(out=ptmp[:, 0], in_=pvs(14), func=AF.Exp, scale=-1.0), [pv_b], [ptmp_b])
        P.op(act, lambda e: e.activation(out=ptmp[:, 1], in_=ptmp[:, 0], func=AF.Ln, bias=1.0, scale=1.0), [ptmp_b], [ptmp_b])
        P.op(dve, lambda e: e.tensor_scalar(out=dvs(6), in0=ptmp[:, 1], scalar1=-4.0, scalar2=None, op0=ALU.mult), [ptmp_b], [dv_b])
        P.op(dve, lambda e: e.tensor_scalar(out=dvs(7), in0=ptmp[:, 1], scalar1=-8.0, scalar2=None, op0=ALU.mult), [ptmp_b], [dv_b])
        lbx = pv[:, 15]
        mx = ptmp[:, 2, 0, :]
        P.op(dve, lambda e: e.tensor_copy(out=mx, in_=lbx[:, 0, :]), [pv_b], [ptmp_b])
        for l in range(1, L):
            P.op(dve, lambda e, l=l: e.tensor_tensor(out=mx, in0=mx, in1=lbx[:, l, :], op=ALU.max), [pv_b, ptmp_b], [ptmp_b])
        for l in range(L):
            P.op(dve, lambda e, l=l: e.tensor_tensor(out=ptmp[:, 3, l, :], in0=lbx[:, l, :], in1=mx, op=ALU.subtract), [pv_b, ptmp_b], [ptmp_b])
        P.op(act, lambda e: e.activation(out=ptmp[:, 4], in_=ptmp[:, 3], func=AF.Exp), [ptmp_b], [ptmp_b])
        sm = ptmp[:, 2, 1 % L if L > 1 else 0, :]
        sm = ptmp[:, 5, 0, :]
        P.op(dve, lambda e: e.tensor_copy(out=sm, in_=ptmp[:, 4, 0, :]), [ptmp_b], [ptmp_b])
        for l in range(1, L):
            P.op(dve, lambda e, l=l: e.tensor_tensor(out=sm, in0=sm, in1=ptmp[:, 4, l, :], op=ALU.add), [ptmp_b], [ptmp_b])
        if L > 1:
            rs = ptmp[:, 5, 1, :]
        else:
            rs = ptmp[:, 2, 0, :]
        P.op(dve, lambda e: e.reciprocal(out=rs, in_=sm), [ptmp_b], [ptmp_b])
        for l in range(L):
            P.op(dve, lambda e, l=l: e.tensor_tensor(out=ptmp[:, 3, l, :], in0=ptmp[:, 4, l, :], in1=rs, op=ALU.mult), [ptmp_b], [ptmp_b])
        P.op(dve, lambda e: e.memset(ptmp[:, 4, 0, :], 0.0), [ptmp_b], [ptmp_b])
        for l in range(1, L):
            P.op(dve, lambda e, l=l: e.tensor_tensor(out=ptmp[:, 4, l, :], in0=ptmp[:, 4, l - 1, :], in1=ptmp[:, 3, l, :], op=ALU.add), [ptmp_b], [ptmp_b])
        P.op(dve, lambda e: e.tensor_scalar(out=dvs(8), in0=ptmp[:, 4], scalar1=-0.5, scalar2=0.5, op0=ALU.mult, op1=ALU.add), [ptmp_b], [dv_b])
        P.op(dve, lambda e: e.tensor_scalar(out=dvs(9), in0=ptmp[:, 4], scalar1=0.5, scalar2=0.5, op0=ALU.mult, op1=ALU.add), [ptmp_b], [dv_b])
        P.op(dve, lambda e: e.tensor_scalar(out=dvs(10), in0=ptmp[:, 4], scalar1=0.5, scalar2=-0.5, op0=ALU.mult, op1=ALU.add), [ptmp_b], [dv_b])

        stream = {"issued": 0, "consumed": 0, "released": 0}
        units = [(b, j, l) for b in range(NSEQ) for j in range(NTILE) for l in range(L)]
        total_groups = len(units) * NG

        def issue_loads(upto):
            while stream["issued"] < min(upto, total_groups):
                k = stream["issued"]
                u, g = divmod(k, NG)
                l = units[u][2]
                s = k % NSLOT
                P.dma(sp, wsem[s], wring[:, s, 0:GLEN[g]], wb_d[l, g, :, 0:GLEN[g]], [scr_b[l][g]], [wring_b[s]])
                stream["issued"] += 1

        def next_group():
            k = stream["consumed"]
            issue_loads(stream["released"] + NSLOT)
            assert k < stream["issued"], (k, stream)
            stream["consumed"] += 1
            s = k % NSLOT
            return wring[:, s, :].rearrange("p (b k c) -> p b k c", b=4, k=8), wring_b[s], wring[:, s, :]

        def release(n=1):
            stream["released"] += n
            assert stream["released"] <= stream["consumed"]
            issue_loads(stream["released"] + NSLOT)

        def mm_fm(w, wb, blk, rhs_ap, rhs_b, n=T, reserve=False):
            pa, pb = bank(reserve)
            for kc in range(KC):
                P.op(pe, lambda e, kc=kc: e.matmul(pa[:, 0:n], lhsT=w[:, blk, kc, :], rhs=rhs_ap(kc),
                                                   start=(kc == 0), stop=(kc == KC - 1)),
                     [wb, rhs_b[kc]], [pb])
            return pa, pb

        def mm_tm(w, wb, tb, lhs, lhs_b):
            pa, pb = bank()
            for kc in range(KC):
                P.op(pe, lambda e, kc=kc: e.matmul(pa, lhsT=lhs[:, kc, tb * 128:(tb + 1) * 128], rhs=w[:, :, kc, :],
                                                   start=(kc == 0), stop=(kc == KC - 1)),
                     [wb, lhs_b[kc]], [pb])
            return pa, pb

        def rms_rstd(src_ap, src_bufs, ones_idx, nchunks, from_psum=False):
            pa, pb = bank()
            for c in range(nchunks):
                sq, sqb = T16()
                P.op(act, lambda e, c=c, sq=sq: e.activation(out=sq, in_=src_ap(c), func=AF.Square), [src_bufs[c]], [sqb])
                P.op(pe, lambda e, c=c, sq=sq: e.matmul(pa, lhsT=ones[:, ones_idx, :], rhs=sq, start=(c == 0), stop=(c == nchunks - 1)),
                     [sqb, constb], [pb])
            sd, sdb = T32()
            if DEBUG["stage"] == 0.2:
                return sd, sdb
            P.op(act, lambda e: e.activation(out=sd, in_=pa, func=AF.Sqrt, bias=EPS, scale=1.0), [pb], [sdb])
            rstd, rb = T32()
            if DEBUG["stage"] == 0.4:
                return sd, sdb
            P.op(dve, lambda e: e.reciprocal(out=rstd, in_=sd), [sdb], [rb])
            return rstd, rb

        def gate_merge(l, br, ypa, ypb, mpa, mpb, d):
            th, thb = T32()
            P.op(act, lambda e: e.activation(out=th, in_=mpa, func=AF.Tanh, bias=dv[:, 1 + br, l, d:d + 1], scale=0.5), [mpb, dv_b], [thb])
            if DEBUG["stage"] == 1.7:
                return
            if br == DEBUG["branches"].find("abc"[br]) == 0 or "abc"[br] == DEBUG["branches"][0]:
                P.op(dve, lambda e: e.scalar_tensor_tensor(out=merged[:, d, :], in0=th, scalar=1.0, in1=ypa, op0=ALU.add, op1=ALU.mult),
                     [thb, ypb], [merged_b[d]])
            else:
                tm, tmb = T32()
                P.op(dve, lambda e: e.scalar_tensor_tensor(out=tm, in0=th, scalar=1.0, in1=ypa, op0=ALU.add, op1=ALU.mult),
                     [thb, ypb], [tmb])
                P.op(pool, lambda e: e.tensor_tensor(out=merged[:, d, :], in0=merged[:, d, :], in1=tm, op=ALU.add),
                     [tmb, merged_b[d]], [merged_b[d]])

        def proj_and_merge(l, br, yin, yin_b):
            for half in range(2):
                wm, wmb, _ = next_group()
                wp, wpb, _ = next_group()
                for i in range(4):
                    d = half * 4 + i
                    mpa, mpb = mm_fm(wm, wmb, i, lambda kc: xn[:, kc, :], xn_b)
                    ypa, ypb = mm_fm(wp, wpb, i, lambda kc: yin[:, kc, :], yin_b)
                    if "abc"[br] in DEBUG["branches"] and DEBUG["stage"] != 1.6:
                        gate_merge(l, br, ypa, ypb, mpa, mpb, d)
                release(2)

        def unit(l):
            rstd, rb = rms_rstd(lambda c: xres[:, c, :], xres_b, 0, KC)
            if DEBUG["stage"] < 0.8:
                return
            for c in range(KC):
                P.op(dve, lambda e, c=c: e.scalar_tensor_tensor(out=xn[:, c, :], in0=xres[:, c, :], scalar=pv[:, 0, l, c:c + 1], in1=rstd,
                                                                  op0=ALU.mult, op1=ALU.mult),
                     [xres_b[c], rb, pv_b], [xn_b[c]])

            if DEBUG["stage"] < 1:
                return
            nbf, nbf_b = big[0], big_b[0]
            nview = nbf[:].rearrange("p a t -> p (a t)").rearrange("p (tb n) -> p tb n", tb=4)
            wv = [next_group(), next_group()]
            for tb in range(4):
                vi = 0
                ctr["vt"] += 1
                for half in range(2):
                    pa, pb = mm_tm(wv[half][0], wv[half][1], tb, xn, xn_b)
                    P.op(act, lambda e, pa=pa, half=half, vi=vi: e.activation(out=vtok[:, vi, half * 512:(half + 1) * 512], in_=pa, func=AF.Gelu_apprx_tanh),
                         [pb], [vtok_b[vi]])
                if DEBUG["stage"] == 1.1:
                    continue
                st6 = stat[:, tb, 0:12]
                for half in range(2):
                    P.op(dve, lambda e, half=half, vi=vi: e.bn_stats(out=stat[:, tb, half * 6:(half + 1) * 6], in_=vtok[:, vi, half * 512:(half + 1) * 512]),
                         [vtok_b[vi]], [stat_b[tb]])
                P.op(dve, lambda e: e.bn_aggr(out=stat[:, tb, 12:14], in_=st6), [stat_b[tb]], [stat_b[tb]])
                P.op(act, lambda e: e.activation(out=stat[:, tb, 14:15], in_=stat[:, tb, 13:14], func=AF.Sqrt, bias=EPS, scale=1.0), [stat_b[tb]], [stat_b[tb]])
                P.op(dve, lambda e: e.reciprocal(out=stat[:, tb, 15:16], in_=stat[:, tb, 14:15]), [stat_b[tb]], [stat_b[tb]])
                P.op(dve, lambda e, vi=vi: e.tensor_scalar(out=nview[:, tb, :], in0=vtok[:, vi, :], scalar1=stat[:, tb, 12:13], scalar2=stat[:, tb, 15:16],
                                                           op0=ALU.subtract, op1=ALU.mult),
                     [vtok_b[vi], stat_b[tb]], nbf_b)
            release(2)
            if DEBUG["stage"] in (1.1, 1.2):
                return
            _, auxb, auxflat = next_group()
            wsT = auxflat[:, 0:1024].rearrange("p (g t) -> p g t", g=8)
            P.op(pool, lambda e: e.tensor_tensor(out=wsm[:], in0=wsT, in1=mask_tril.unsqueeze(1).to_broadcast([128, 8, 128]), op=ALU.mult),
                 [auxb, cst_b], [wsm_b])
            release(1)
            if DEBUG["stage"] == 1.3:
                return
            P.dma(sp, m2sem, m2[:].rearrange("p g t -> p (g t)"), bs_d[l:l + 1, :].partition_broadcast(128), [], [m2_b])
            for hf in range(2):
                pa, pb = bank()
                P.op(pe, lambda e, pa=pa, hf=hf: e.matmul(pa, lhsT=ones[:, 3, :], rhs=wsm[:, hf * 4:(hf + 1) * 4, :], start=True, stop=True),
                     [wsm_b, constb], [pb])
                for gg in range(4):
                    g = hf * 4 + gg
                    P.op(dve, lambda e, pa=pa, gg=gg, g=g: e.scalar_tensor_tensor(out=m2[:, g, :], in0=pa[:, gg * 128:(gg + 1) * 128], scalar=pv[:, 6, l, g:g + 1],
                                                                                  in1=m2[:, g, :], op0=ALU.mult, op1=ALU.add),
                         [pb, m2_b, pv_b], [m2_b])
            if DEBUG["stage"] == 1.4:
                return
            yin, yin_b = big[1], big_b[1]
            for half in range(2):
                wu, wub, _ = next_group()
                wg, wgb, _ = next_group()
                for i in range(4):
                    c = half * 4 + i
                    upa, upb = mm_fm(wu, wub, i, lambda kc: xn[:, kc, :], xn_b)
                    ug, ugb = T16()
                    P.op(act, lambda e: e.activation(out=ug, in_=upa, func=AF.Gelu_apprx_tanh), [upb], [ugb])
                    gpa, gpb = mm_fm(wg, wgb, i, lambda kc: xn[:, kc, :], xn_b)
                    sg, sgb = T16()
                    P.op(act, lambda e: e.activation(out=sg, in_=gpa, func=AF.Silu), [gpb], [sgb])
                    spa, spb = bank()
                    for tb in range(4):
                        P.op(pe, lambda e, tb=tb: e.matmul(spa[:, tb * 128:(tb + 1) * 128], lhsT=nview[:, tb, c * 128:(c + 1) * 128], rhs=wsm[:, c, :],
                                                           start=True, stop=True),
                             nbf_b + [wsm_b], [spb])
                    mx_, mxb = T32()
                    P.op(dve, lambda e: e.scalar_tensor_tensor(out=mx_.rearrange("p (a t) -> p a t", a=4), in0=spa.rearrange("p (a t) -> p a t", a=4),
                                                               scalar=pv[:, 5, l, c:c + 1],
                                                               in1=m2[:, c, :].unsqueeze(1).to_broadcast([128, 4, 128]), op0=ALU.mult, op1=ALU.add),
                         [spb, m2_b, pv_b], [mxb])
                    P.op(pool, lambda e: e.tensor_tensor(out=ug, in0=ug, in1=sg, op=ALU.mult), [ugb, sgb], [ugb])
                    P.op(pool, lambda e: e.tensor_tensor(out=yin[:, c, :], in0=mx_, in1=ug, op=ALU.mult), [mxb, ugb], [yin_b[c]])
                release(2)
            if DEBUG["stage"] == 1.5:
                return
            proj_and_merge(l, 0, yin, yin_b)

            if DEBUG["stage"] < 2:
                return
            _, auxb, auxflat = next_group()
            P.op(pool, lambda e: e.tensor_copy(out=wri[:], in_=auxflat[:, 0:2048]), [auxb], [wri_b])
            release(1)
            auxb = wri_b
            wr = wri[:, 0:1024].rearrange("p (h j) -> p h j", h=8)
            wi = wri[:, 1024:2048].rearrange("p (h j) -> p h j", h=8)
            P.op(pool, lambda e: e.tensor_copy(out=xl[:, :, 0:3], in_=xtail[:, l]), [xtail_b[l]], xl_b)
            yin, yin_b = big[2], big_b[2]
            for half in range(2):
                wx, wxb, _ = next_group()
                wg, wgb, _ = next_group()
                for k in range(4):
                    P.op(pool, lambda e, k=k: e.tensor_tensor(out=dg[:, k], in0=ident.unsqueeze(1).to_broadcast([128, 4, 128]),
                                                               in1=pv[:, 7 + k, l, half * 4:half * 4 + 4].unsqueeze(2).to_broadcast([128, 4, 128]), op=ALU.mult),
                         [cst_b, pv_b], [dg_b])
                for pair in range(2):
                    ii = [pair * 2, pair * 2 + 1]
                    cc_ = [half * 4 + i for i in ii]
                    st_ = {}
                    for i, c in zip(ii, cc_):
                        xpa, xpb = mm_fm(wx, wxb, i, lambda kc: xn[:, kc, :], xn_b)
                        P.op(dve, lambda e: e.tensor_copy(out=xl[:, c, 3:3 + T], in_=xpa), [xpb], [xl_b[c]])
                    for i, c in zip(ii, cc_):
                        cpa, cpb = bank()
                        for k in range(4):
                            P.op(pe, lambda e, k=k: e.matmul(cpa, lhsT=dg[:, k, i, :], rhs=xl[:, c, k:k + T], start=(k == 0), stop=(k == 3)),
                                 [dg_b, xl_b[c]], [cpb])
                        xbf, xbfb = T32()
                        P.op(act, lambda e: e.activation(out=xbf, in_=cpa, func=AF.Identity, bias=pv[:, 11, l, c:c + 1], scale=1.0), [cpb, pv_b], [xbfb])
                        xbb, xbbb = T16()
                        P.op(dve, lambda e: e.tensor_scalar(out=xbb, in0=cpa, scalar1=pv[:, 11, l, c:c + 1], scalar2=None, op0=ALU.add), [cpb, pv_b], [xbbb])
                        st_[c] = {"xbf": (xbf, xbfb), "xbb": (xbb, xbbb)}
                    for i, c in zip(ii, cc_):
                        xbb, xbbb = st_[c]["xbb"]
                        rpa, rpb = bank()
                        P.op(pe, lambda e: e.matmul(rpa, lhsT=wr[:, c, :], rhs=xbb, start=True, stop=True), [auxb, xbbb], [rpb])
                        ipa, ipb = bank()
                        P.op(pe, lambda e: e.matmul(ipa, lhsT=wi[:, c, :], rhs=xbb, start=True, stop=True), [auxb, xbbb], [ipb])
                        st_[c]["r"] = (rpa, rpb)
                        st_[c]["i"] = (ipa, ipb)
                    for i, c in zip(ii, cc_):
                        rpa, rpb = st_[c]["r"]
                        ipa, ipb = st_[c]["i"]
                        thr, thrb = T32()
                        P.op(act, lambda e: e.activation(out=thr, in_=rpa, func=AF.Tanh, bias=dv[:, 4, l, c:c + 1], scale=0.5), [rpb, dv_b], [thrb])
                        thi, thib = T32()
                        P.op(act, lambda e: e.activation(out=thi, in_=ipa, func=AF.Tanh, bias=dv[:, 5, l, c:c + 1], scale=0.5), [ipb, dv_b], [thib])
                        st_[c]["thr"] = (thr, thrb)
                        st_[c]["thi"] = (thi, thib)
                    for i, c in zip(ii, cc_):
                        st_[c]["g"] = mm_fm(wg, wgb, i, lambda kc: xn[:, kc, :], xn_b)
                    for i, c in zip(ii, cc_):
                        thr, thrb = st_[c]["thr"]
                        a2, a2b = T32()
                        P.op(act, lambda e: e.activation(out=thr, in_=thr, func=AF.Exp, bias=dv[:, 6, l, c:c + 1], scale=dv[:, 6, l, c:c + 1]), [thrb, dv_b], [thrb])
                        P.op(pool, lambda e: e.tensor_tensor(out=a2, in0=thr, in1=thr, op=ALU.mult), [thrb], [a2b])
                        st_[c]["a2"] = (a2, a2b)
                    for i, c in zip(ii, cc_):
                        a2, a2b = st_[c]["a2"]
                        P.op(act, lambda e: e.activation(out=a2, in_=a2, func=AF.Sqrt, bias=0.25, scale=-0.25), [a2b], [a2b])
                    for i, c in zip(ii, cc_):
                        gpa, gpb = st_[c]["g"]
                        sg, sgb = T16()
                        P.op(act, lambda e: e.activation(out=sg, in_=gpa, func=AF.Silu), [gpb], [sgb])
                        st_[c]["sg"] = (sg, sgb)
                    for i, c in zip(ii, cc_):
                        xbf, xbfb = st_[c]["xbf"]
                        thi, thib = st_[c]["thi"]
                        aa, aab = st_[c]["thr"]
                        a2, a2b = st_[c]["a2"]
                        sg, sgb = st_[c]["sg"]
                        P.op(dve, lambda e: e.scalar_tensor_tensor(out=thi, in0=thi, scalar=1.0, in1=xbf, op0=ALU.add, op1=ALU.mult), [thib, xbfb], [thib])
                        P.op(pool, lambda e: e.tensor_tensor(out=thi, in0=thi, in1=a2, op=ALU.mult), [thib, a2b], [thib])
                        P.op(dve, lambda e: e.tensor_tensor_scan(out=xbf, data0=aa, data1=thi, initial=lruh[:, l, c:c + 1], op0=ALU.mult, op1=ALU.add),
                             [aab, thib, lruh_b[l][c]], [xbfb])
                        P.op(pool, lambda e: e.tensor_copy(out=lruh[:, l, c:c + 1], in_=xbf[:, T - 1:T]), [xbfb], [lruh_b[l][c]])
                        P.op(pool, lambda e: e.tensor_tensor(out=yin[:, c, :], in0=xbf, in1=sg, op=ALU.mult), [xbfb, sgb], [yin_b[c]])
                release(2)
            P.op(pool, lambda e: e.tensor_copy(out=xtail[:, l], in_=xl[:, :, T:T + 3]), xl_b, [xtail_b[l]])
            proj_and_merge(l, 1, yin, yin_b)

            if DEBUG["stage"] < 3:
                return
            vt_, vt_b = big[0], big_b[0]
            vview = vt_[:].rearrange("p a t -> p (a t)").rearrange("p (tb n) -> p tb n", tb=4)
            wv = [next_group(), next_group()]
            for tb in range(4):
                for half in range(2):
                    pa, pb = mm_tm(wv[half][0], wv[half][1], tb, xn, xn_b)
                    P.op(act, lambda e, pa=pa, half=half, tb=tb: e.activation(out=vview[:, tb, half * 512:(half + 1) * 512], in_=pa, func=AF.Copy),
                         [pb], vt_b)
            release(2)
            yin, yin_b = big[1], big_b[1]
            qg_, qg_b = big[2], big_b[2]
            kd_, kd_b = big[3], big_b[3]
            for hg in range(2):
                wq, wqb, _ = next_group()
                wf, wfb, _ = next_group()
                for pair in range(2):
                    ii = [pair * 2, pair * 2 + 1]
                    st_ = {}
                    for i in ii:
                        fpa, fpb = mm_fm(wf, wfb, i, lambda kc: xn[:, kc, :], xn_b)
                        th, thb = T32()
                        P.op(act, lambda e: e.activation(out=th, in_=fpa, func=AF.Tanh, scale=0.5), [fpb], [thb])
                        st_[i] = {"th": (th, thb)}
                    for i in ii:
                        st_[i]["q"] = mm_fm(wq, wqb, i, lambda kc: xn[:, kc, :], xn_b)
                    for i in ii:
                        h = hg * 4 + i
                        th, thb = st_[i]["th"]
                        ff, ffb = T32()
                        P.op(dve, lambda e: e.tensor_scalar(out=ff, in0=th, scalar1=dv[:, 8, l, h:h + 1], scalar2=dv[:, 9, l, h:h + 1], op0=ALU.mult, op1=ALU.add),
                             [thb, dv_b], [ffb])
                        G, Gb = T32()
                        P.op(dve, lambda e: e.tensor_tensor_scan(out=G, data0=rmask, data1=ff, initial=1.0, op0=ALU.max, op1=ALU.mult), [ffb, cst_b], [Gb])
                        P.op(pool, lambda e: e.tensor_copy(out=glast[:, i, :], in_=G.rearrange("p (c t) -> p c t", t=32)[:, :, 31]), [Gb], [glast_b[i]])
                        rG = ff
                        P.op(dve, lambda e: e.reciprocal(out=rG, in_=G), [Gb, ffb], [ffb])
                        P.op(pool, lambda e: e.tensor_scalar(out=th, in0=th, scalar1=dv[:, 10, l, h:h + 1], scalar2=dv[:, 8, l, h:h + 1], op0=ALU.mult, op1=ALU.add),
                             [thb, dv_b], [thb])
                        kg, kgb = T16()
                        P.op(pool, lambda e: e.tensor_tensor(out=kg, in0=th, in1=rG, op=ALU.mult), [thb, ffb], [kgb])
                        kdec, kdecb = T16()
                        P.op(pool, lambda e: e.tensor_tensor(out=kdec.rearrange("p (c t) -> p c t", t=32), in0=kg.rearrange("p (c t) -> p c t", t=32),
                                                             in1=glast[:, i, :].unsqueeze(2).to_broadcast([128, 16, 32]), op=ALU.mult),
                             [kgb, glast_b[i]], [kdecb])
                        qpa, qpb = st_[i]["q"]
                        P.op(dve, lambda e: e.tensor_tensor(out=qg_[:, i, :], in0=qpa, in1=G, op=ALU.mult), [qpb, Gb], [qg_b[i]])
                        st_[i]["kg"] = (kg, kgb)
                        st_[i]["kdec"] = (kdec, kdecb)
                    for i in ii:
                        kg, kgb = st_[i]["kg"]
                        kdec, kdecb = st_[i]["kdec"]
                        spa, spb = bank()
                        for tb in range(4):
                            P.op(pe, lambda e, tb=tb: e.matmul(spa[:, tb * 128:(tb + 1) * 128], lhsT=kg[:, tb * 128:(tb + 1) * 128],
                                                               rhs=qg_[:, i, tb * 128:(tb + 1) * 128], start=True, stop=True),
                                 [kgb, qg_b[i]], [spb])
                        P.op(dve, lambda e: e.tensor_tensor(out=kd_[:, 4 + i, :], in0=spa, in1=mask_ch, op=ALU.mult), [spb, cst_b], [kd_b[4 + i]])
                        tpa, tpb = bank()
                        tpbf = tpa.bitcast(BF16)
                        for tb in range(4):
                            P.op(pe, lambda e, tb=tb: e.transpose(tpbf[:, tb * 128:(tb + 1) * 128], kdec[:, tb * 128:(tb + 1) * 128], identb[:]),
                                 [kdecb, constb], [tpb])
                        P.op(act, lambda e: e.activation(out=kd_[:, i, :], in_=tpbf[:, 0:512], func=AF.Copy), [tpb], [kd_b[i]])
                release(2)
                wgc, wgcb, _ = next_group()
                for i in range(4):
                    h = hg * 4 + i
                    P.op(act, lambda e, i=i, h=h: e.activation(out=sall[:, 0, i * 128:(i + 1) * 128], in_=hgst[:, l, h * 128:(h + 1) * 128], func=AF.Copy),
                         [hgst_b[l][h]], [sall_b[0]])
                sgs = {}
                for c in range(16):
                    tb, cc = divmod(c, 4)
                    upa, upb = bank()
                    for i in range(4):
                        h = hg * 4 + i
                        P.op(pe, lambda e, i=i, h=h: e.matmul(upa[:, i * 128:(i + 1) * 128], lhsT=kd_[cc * 32:(cc + 1) * 32, i, tb * 128:(tb + 1) * 128],
                                                             rhs=vview[cc * 32:(cc + 1) * 32, tb, h * 128:(h + 1) * 128], start=True, stop=True,
                                                             tile_position=(cc * 32, 0)),
                             [kd_b[i]] + vt_b, [upb])
                    for i in range(4):
                        h = hg * 4 + i
                        if c == 0:
                            src, srcb = hgst[:, l, h * 128:(h + 1) * 128], hgst_b[l][h]
                        else:
                            src, srcb = stw[:, c % 2, i * 128:(i + 1) * 128], stw_b[c % 2][i]
                        if c == 15:
                            dst, dstb = hgst[:, l, h * 128:(h + 1) * 128], hgst_b[l][h]
                        else:
                            dst, dstb = stw[:, (c + 1) % 2, i * 128:(i + 1) * 128], stw_b[(c + 1) % 2][i]
                        P.op(dve, lambda e, i=i, src=src, dst=dst: e.scalar_tensor_tensor(out=dst, in0=src, scalar=glast[:, i, c:c + 1], in1=upa[:, i * 128:(i + 1) * 128],
                                                                                         op0=ALU.mult, op1=ALU.add),
                             [srcb, glast_b[i], upb], [dstb])
                        if c < 15:
                            P.op(act, lambda e, i=i, dst=dst: e.activation(out=sall[:, c + 1, i * 128:(i + 1) * 128], in_=dst, func=AF.Copy),
                                 [dstb], [sall_b[c + 1]])
                    if c % 4 == 3:
                        i = c // 4
                        sgs[i] = mm_fm(wgc, wgcb, i, lambda kc: xn[:, kc, :], xn_b, reserve=True)
                release(1)
                for i in range(4):
                    h = hg * 4 + i
                    opa, opb = bank()
                    for tb in range(4):
                        P.op(pe, lambda e, tb=tb, h=h, i=i: e.matmul(opa[:, tb * 128:(tb + 1) * 128], lhsT=vview[:, tb, h * 128:(h + 1) * 128],
                                                                     rhs=kd_[:, 4 + i, tb * 128:(tb + 1) * 128], start=True, stop=False),
                             vt_b + [kd_b[4 + i]], [opb])
                        for cc in range(4):
                            c = tb * 4 + cc
                            P.op(pe, lambda e, c=c, i=i, cc=cc: e.matmul(opa[:, c * 32:(c + 1) * 32], lhsT=sall[:, c, i * 128:(i + 1) * 128],
                                                                         rhs=qg_[:, i, c * 32:(c + 1) * 32], start=False, stop=(cc == 3)),
                                 [sall_b[c], qg_b[i]], [opb])
                    rstd, rb = rms_rstd(lambda c_: opa, [opb], 2, 1)
                    on, onb = T32()
                    P.op(dve, lambda e: e.scalar_tensor_tensor(out=on, in0=opa, scalar=pv[:, 16, l, h:h + 1], in1=rstd, op0=ALU.mult, op1=ALU.mult),
                         [opb, rb, pv_b], [onb])
                    gpa, gpb = sgs[i]
                    sg, sgb = T16()
                    P.op(act, lambda e: e.activation(out=sg, in_=gpa, func=AF.Silu), [gpb], [sgb])
                    resv.discard(ps_b.index(gpb))
                    P.op(pool, lambda e: e.tensor_tensor(out=yin[:, h, :], in0=on, in1=sg, op=ALU.mult), [onb, sgb], [yin_b[h]])
            proj_and_merge(l, 2, yin, yin_b)

            if DEBUG["stage"] < 4:
                return
            mbf, mbf_b = big[2], big_b[2]
            for c in range(KC):
                P.op(act, lambda e, c=c: e.activation(out=mbf[:, c, :], in_=merged[:, c, :], func=AF.Copy), [merged_b[c]], [mbf_b[c]])
            ppa, ppb = bank(reserve=True)
            for half in range(2):
                wo, wob, _ = next_group()
                for i in range(4):
                    d = half * 4 + i
                    ypa, ypb = mm_fm(wo, wob, i, lambda kc: mbf[:, kc, :], mbf_b)
                    sq, sqb = T16()
                    P.op(act, lambda e: e.activation(out=sq, in_=ypa, func=AF.Square), [ypb], [sqb])
                    P.op(pe, lambda e, d=d, sq=sq: e.matmul(ppa, lhsT=ones[:, 1, :], rhs=sq, start=(d == 0), stop=(d == 7)), [sqb, constb], [ppb])
                    P.op(dve, lambda e, d=d: e.tensor_scalar(out=merged[:, d, :], in0=ypa, scalar1=dv[:, 0, l, d:d + 1], scalar2=None, op0=ALU.mult),
                         [ypb, dv_b], [merged_b[d]])
                release(1)
            resv.clear()
            sd, sdb = T32()
            P.op(act, lambda e: e.activation(out=sd, in_=ppa, func=AF.Sqrt, bias=EPS, scale=1.0), [ppb], [sdb])
            rstd, rb = T32()
            P.op(dve, lambda e: e.reciprocal(out=rstd, in_=sd), [sdb], [rb])
            for d in range(KC):
                P.op(dve, lambda e, d=d: e.tensor_tensor(out=merged[:, d, :], in0=merged[:, d, :], in1=rstd, op=ALU.mult), [merged_b[d], rb], [merged_b[d]])
                P.op(pool, lambda e, d=d: e.tensor_tensor(out=xres[:, d, :], in0=xres[:, d, :], in1=merged[:, d, :], op=ALU.add),
                     [merged_b[d], xres_b[d]], [xres_b[d]])

        xio = merged[:].rearrange("p a t -> p (a t)").rearrange("p (tb n) -> p tb n", tb=4)
        for b in range(NSEQ):
            if b > 0:
                P.op(dve, lambda e: e.memset(hgst[:].rearrange("p l n -> p (l n)"), 0.0), [], [bb for bl in hgst_b for bb in bl])
                P.op(dve, lambda e: e.memset(lruh[:].rearrange("p l n -> p (l n)"), 0.0), [], [bb for bl in lruh_b for bb in bl])
                P.op(dve, lambda e: e.memset(xtail[:].rearrange("p l c k -> p (l c k)"), 0.0), [], xtail_b)
            for j in range(NTILE):
                P.dma(pool, iosem, xio, x_d[b, j * T:(j + 1) * T, :].rearrange("(tb p) n -> p tb n", p=128), [], merged_b)
                for c in range(KC):
                    pa, pb = bank()
                    for tb in range(4):
                        P.op(pe, lambda e, tb=tb, c=c: e.transpose(pa[:, tb * 128:(tb + 1) * 128], xio[:, tb, c * 128:(c + 1) * 128], ident),
                             merged_b + [cst_b], [pb])
                    P.op(act, lambda e, c=c, pa=pa: e.activation(out=xres[:, c, :], in_=pa, func=AF.Copy), [pb], [xres_b[c]])
                for l in range(L):
                    unit(l)
                for tb in range(4):
                    for hf in range(2):
                        pa, pb = bank()
                        for cc in range(4):
                            c = hf * 4 + cc
                            P.op(pe, lambda e, cc=cc, c=c, tb=tb: e.transpose(pa[:, cc * 128:(cc + 1) * 128], xres[:, c, tb * 128:(tb + 1) * 128], ident),
                                 [xres_b[c], cst_b], [pb])
                        P.op(act, lambda e, pa=pa, tb=tb, hf=hf: e.activation(out=xio[:, tb, hf * 512:(hf + 1) * 512], in_=pa, func=AF.Copy), [pb], merged_b)
                P.dma(pool, iosem, out_d[b, j * T:(j + 1) * T, :].rearrange("(tb p) n -> p tb n", p=128), xio, merged_b, [])
        if P.dry:
            return P.waited
        nc.gpsimd.wait_ge(iosem[0], iosem[1])
        stats = {e.name: (e.n, e.ninc) for e in (pe, act, dve, pool)}
        print("instr counts (ops, sem incs)", stats, "groups", stream)
    return nc


def _colblock(w, b0):
    sub = w[:, b0 * 128:(b0 + 4) * 128]
    return sub.reshape(8, 128, 4, 128).transpose(1, 2, 0, 3)


def make_consts():
    cst = np.zeros((128, 1280), np.float32)
    cst[:, 0:128] = np.eye(128, dtype=np.float32)
    s = np.arange(128)[:, None]
    t = np.arange(128)[None, :]
    cst[:, 128:256] = (s <= t).astype(np.float32)
    mch = ((s // 32 == t // 32) & (s <= t)).astype(np.float32)
    cst[:, 256:768] = np.tile(mch, (1, 4))
    rm = np.zeros(512, np.float32)
    rm[::32] = 1.0
    cst[:, 768:1280] = rm[None, :]
    return cst


def prep_shared(inp, L):
    ws = np.empty((L, NG, 128, GSZ), np.float32)
    proj = {"a": inp["w_a_proj"], "b": inp["w_b_proj"], "c": inp["w_c_proj"], "o": inp["w_out"]}
    for l in range(L):
        for g, (kind, b0) in enumerate(GROUPS):
            if kind == "in":
                ws[l, g] = _colblock(inp["w_in"][l], b0).reshape(128, GSZ)
            elif kind == "auxA":
                ws[l, g] = 0.0
                ws[l, g, :, 0:1024] = inp["gm_w_s"][l].transpose(2, 0, 1).reshape(128, 1024)
            elif kind == "auxB":
                ws[l, g] = 0.0
                ws[l, g, :, 0:1024] = inp["lru_w_r"][l].transpose(1, 0, 2).reshape(128, 1024)
                ws[l, g, :, 1024:2048] = inp["lru_w_i"][l].transpose(1, 0, 2).reshape(128, 1024)
            else:
                ws[l, g] = _colblock(proj[kind][l], b0).reshape(128, GSZ)
    names = [("pre_norm_g", None), ("post_norm_g", None), ("b_merge", 0), ("b_merge", 1), ("b_merge", 2),
             ("gm_ln_g", None), ("gm_ln_b", None), ("lru_conv_w", 0), ("lru_conv_w", 1), ("lru_conv_w", 2), ("lru_conv_w", 3),
             ("lru_conv_b", None), ("lru_b_r", None), ("lru_b_i", None), ("lru_lambda", None), ("hg_lower_bounds", None), ("hg_norm_g", None)]
    pvec = np.empty((128, NPV, L, 8), np.float32)
    for i, (n, k) in enumerate(names):
        a = inp[n][:L] if k is None else inp[n][:L, k]
        pvec[:, i] = a.reshape(L, 8, 128).transpose(2, 0, 1)
    bsrow = np.ascontiguousarray(inp["gm_b_s"][:L].reshape(L, 1024))
    return {"wstream": ws, "pvec": pvec.reshape(128, -1), "bsrow": bsrow, "cst": make_consts()}


def run(inputs, L, n_cores, nseq_per_core, ntile):
    inp = {k: np.asarray(v) for k, v in inputs.items()}
    shared = prep_shared(inp, L)
    need = build_program(L, nseq_per_core, ntile, None)
    nc = build_program(L, nseq_per_core, ntile, need)
    x = np.ascontiguousarray(inp["x"], dtype=np.float32)
    in_maps = []
    for c in range(n_cores):
        m = dict(shared)
        m["x"] = np.ascontiguousarray(x[c * nseq_per_core:(c + 1) * nseq_per_core])
        in_maps.append(m)
    res = run_bass_kernel_spmd(nc, in_maps, core_ids=list(range(n_cores)))
    return np.concatenate([r["out"] for r in res.results], axis=0)


def kernel(**inputs):
    return run(inputs, 4, 8, 4, 4)
```

```python
import numpy as np
import concourse.bass as bass
import concourse.mybir as mybir
from concourse.bass_utils import run_bass_kernel_spmd

F32 = mybir.dt.float32
BF16 = mybir.dt.bfloat16
AF = mybir.ActivationFunctionType
ALU = mybir.AluOpType

D = 1024
KC = 8
T = 512
INC = 12288
NG = 34
GSZ = 4096
EPS = 1e-6
NPV = 17
NDV = 11
NSLOT = 4

CB_U, CB_V, CB_GA, CB_X, CB_GB, CB_Q, CB_F, CB_I, CB_GC, CB_M = 0, 8, 16, 24, 32, 40, 48, 56, 64, 72

GROUPS = [
    ("in", CB_V), ("in", CB_V + 4), ("auxA", 0),
    ("in", CB_U), ("in", CB_GA), ("in", CB_U + 4), ("in", CB_GA + 4),
    ("in", CB_M), ("a", 0), ("in", CB_M + 4), ("a", 4),
    ("auxB", 0), ("in", CB_X), ("in", CB_GB), ("in", CB_X + 4), ("in", CB_GB + 4),
    ("in", CB_M + 8), ("b", 0), ("in", CB_M + 12), ("b", 4),
    ("in", CB_I), ("in", CB_I + 4),
    ("in", CB_Q), ("in", CB_F), ("in", CB_GC), ("in", CB_Q + 4), ("in", CB_F + 4), ("in", CB_GC + 4),
    ("in", CB_M + 16), ("c", 0), ("in", CB_M + 20), ("c", 4),
    ("o", 0), ("o", 4),
]
assert len(GROUPS) == NG
GLEN = [1024 if k == 'auxA' else (2048 if k == 'auxB' else GSZ) for k, _ in GROUPS]


class Buf:
    __slots__ = ("w", "r", "excl")

    def __init__(self, excl=False):
        self.w = None
        self.r = {}
        self.excl = excl


class Eng:
    def __init__(self, name, h, sem, pe=False):
        self.name = name
        self.h = h
        self.sem = sem
        self.n = 0
        self.ninc = 0
        self.comp = {}
        self.seen = {}
        self.pe = pe


class Prog:
    def __init__(self, nc, need):
        self.nc = nc
        self.dry = need is None
        self.need = need
        self.waited = set()
        self.engs = {}
        self.dsems = {}
        self.pool_hook = None

    def _waits(self, eng, reads, writes):
        deps = {}

        def add(d):
            if d is None:
                return
            k = d[0]
            if k not in deps or deps[k] < d[1]:
                deps[k] = d[1]
        for b in reads:
            add(b.w)
        for b in writes:
            add(b.w)
            for d in b.r.items():
                add(d)
        for k, val in deps.items():
            if eng.pe and k == eng.name:
                continue
            if eng.seen.get(k, 0) >= val:
                continue
            eng.seen[k] = val
            if self.dry:
                self.waited.add((k, val))
                continue
            if k in self.engs:
                src = self.engs[k]
                eng.h.wait_ge(src.sem, src.comp[val])
            else:
                eng.h.wait_ge(self.dsems[k], val)

    def _record(self, me, reads, writes):
        for b in reads:
            b.r[me[0]] = me[1]
        for b in writes:
            b.w = me
            b.r = {}

    def op(self, eng, fn, reads, writes):
        ex = [b for b in reads if b.excl]
        if ex:
            reads = [b for b in reads if not b.excl]
            writes = list(writes) + ex
        self._waits(eng, reads, writes)
        eng.n += 1
        if not self.dry:
            ins = fn(eng.h)
            if (eng.name, eng.n) in self.need:
                eng.ninc += 1
                eng.comp[eng.n] = eng.ninc
                ins.then_inc(eng.sem, 1)
        self._record((eng.name, eng.n), reads, writes)
        if self.pool_hook is not None and eng.name == "pool":
            self.pool_hook()

    def dma(self, eng, semc, out, in_, reads, writes):
        self._waits(eng, reads, writes)
        semc[1] += 16
        if not self.dry:
            ins = eng.h.dma_start(out=out, in_=in_)
            ins.then_inc(semc[0], 16)
        self._record((semc[2], semc[1]), reads, writes)


DEBUG = {"branches": "abc", "stage": 9}


def build_program(L, NSEQ, NTILE, need=None):
    nc = bass.Bass("TRN2", target_bir_lowering=False)
    S = NTILE * T
    x_d = nc.dram_tensor("x", [NSEQ, S, D], F32, kind="ExternalInput").ap()
    ws_d = nc.dram_tensor("wstream", [L, NG, 128, GSZ], F32, kind="ExternalInput").ap()
    pv_d = nc.dram_tensor("pvec", [128, NPV * L * 8], F32, kind="ExternalInput").ap()
    bs_d = nc.dram_tensor("bsrow", [L, 1024], F32, kind="ExternalInput").ap()
    cst_d = nc.dram_tensor("cst", [128, 1280], F32, kind="ExternalInput").ap()
    out_d = nc.dram_tensor("out", [NSEQ, S, D], F32, kind="ExternalOutput").ap()
    wb_d = nc.dram_tensor("wbf", [L, NG, 128, GSZ], BF16, kind="Internal").ap()

    import contextlib
    es = contextlib.ExitStack()
    with es:
        def sb(name, shape, dt):
            return es.enter_context(nc.sbuf_tensor("sb_" + name, shape, dt))

        P = Prog(nc, need)

        def sem(name):
            return es.enter_context(nc.semaphore(name))

        def dsem(name):
            h = sem(name)
            P.dsems[name] = h
            return [h, 0, name]

        pe = Eng("pe", nc.tensor, sem("s_pe"), pe=True)
        act = Eng("act", nc.scalar, sem("s_act"))
        dve = Eng("dve", nc.vector, sem("s_dve"))
        pool = Eng("pool", nc.gpsimd, sem("s_pool"))
        sp = Eng("sp", nc.sync, sem("s_sp"))
        for e_ in (pe, act, dve, pool, sp):
            P.engs[e_.name] = e_

        xres = sb("xres", [128, KC, T], F32)
        xres_b = [Buf() for _ in range(KC)]
        merged = sb("merged", [128, KC, T], F32)
        merged_b = [Buf() for _ in range(KC)]
        hgst = sb("hgst", [128, L, 8 * 128], F32)
        hgst_b = [[Buf() for _ in range(8)] for _ in range(L)]
        stw = sb("stw", [128, 2, 4 * 128], F32)
        stw_b = [[Buf() for _ in range(4)] for _ in range(2)]
        lruh = sb("lruh", [128, L, 8], F32)
        lruh_b = [[Buf() for _ in range(8)] for _ in range(L)]
        xl = sb("xl", [128, KC, 3 + T], BF16)
        xl_b = [Buf() for _ in range(KC)]
        xtail = sb("xtail", [128, L, KC, 3], BF16)
        xtail_b = [Buf() for _ in range(L)]
        cst = sb("cst", [128, 1280], F32)
        cst_b = Buf()
        pv = sb("pv", [128, NPV, L, 8], F32)
        pv_b = Buf()
        dv = sb("dv", [128, NDV, L, 8], F32)
        dv_b = Buf()
        ptmp = sb("ptmp", [128, 6, L, 8], F32)
        ptmp_b = Buf()
        identb = sb("identb", [128, 128], BF16)
        ones = sb("ones", [128, 4, 128], BF16)
        constb = Buf()
        wring = sb("wring", [128, NSLOT, GSZ], BF16)
        wring_b = [Buf() for _ in range(NSLOT)]
        wsem = [dsem("w%d" % i) for i in range(NSLOT)]
        xn = sb("xn", [128, KC, T], BF16)
        xn_b = [Buf() for _ in range(KC)]
        NBIG = 4
        big = [sb("big%d" % i, [128, KC, T], BF16) for i in range(NBIG)]
        big_b = [[Buf() for _ in range(KC)] for _ in range(NBIG)]
        sall = sb("sall", [128, 16, 4 * 128], BF16)
        sall_b = [Buf() for _ in range(16)]
        NT32 = 10
        t32 = sb("t32", [128, NT32, T], F32)
        t32_b = [Buf() for _ in range(NT32)]
        NT16 = 6
        t16 = sb("t16", [128, NT16, T], BF16)
        t16_b = [Buf() for _ in range(NT16)]
        sgc = sb("sgc", [128, 4, T], BF16)
        sgc_b = [Buf() for _ in range(4)]
        m2 = sb("m2", [128, 8, 128], F32)
        m2_b = Buf()
        wsm = sb("wsm", [128, 8, 128], BF16)
        wsm_b = Buf()
        wri = sb("wri", [128, 2048], BF16)
        wri_b = Buf()
        dg = sb("dg", [128, 4, 4, 128], BF16)
        dg_b = Buf()
        stat = sb("stat", [128, 4, 16], F32)
        stat_b = [Buf() for _ in range(4)]
        glast = sb("glast", [128, 4, 16], F32)
        glast_b = [Buf() for _ in range(4)]
        ps = es.enter_context(nc.psum_tensor("ps", [128, 8 * 512], F32))
        ps_b = [Buf(excl=True) for _ in range(8)]
        scr_b = [[Buf() for _ in range(NG)] if l == 0 else [Buf()] * NG for l in range(L)]
        cvsem = [[dsem("cv0_%d" % g) for g in range(NG)] if l == 0 else [dsem("cv%d" % l)] * NG for l in range(L)]
        iosem = dsem("io")
        misem = dsem("misc")
        pvsem = dsem("pvs")
        m2sem = dsem("m2s")

        ctr = {"ps": 0, "t32": 0, "t16": 0, "vt": 0}
        xio_v = merged[:].rearrange("p a t -> p (a t)").rearrange("p (tb n) -> p tb n", tb=4)

        resv = set()

        def bank(reserve=False):
            while True:
                i = ctr["ps"] % 8
                ctr["ps"] += 1
                if i not in resv:
                    break
            if reserve:
                resv.add(i)
            return ps[:, i * 512:(i + 1) * 512], ps_b[i]

        def T32():
            i = ctr["t32"] % NT32
            ctr["t32"] += 1
            return t32[:, i, :], t32_b[i]

        def T16():
            i = ctr["t16"] % NT16
            ctr["t16"] += 1
            return t16[:, i, :], t16_b[i]

        pending = {l: list(range(NG)) for l in range(L)}
        hook_state = {"layer": None, "n": 0}

        def cast_one(l):
            g = pending[l].pop(0)
            P.dma(pool, cvsem[l][g], wb_d[l, g, :, 0:GLEN[g]], ws_d[l, g, :, 0:GLEN[g]], [], [scr_b[l][g]])

        def flush_casts(l):
            while pending[l]:
                cast_one(l)

        def pool_hook():
            l = hook_state["layer"]
            if l is None or l >= L or not pending[l]:
                return
            hook_state["n"] += 1
            if hook_state["n"] % 3 == 0:
                hk = P.pool_hook
                P.pool_hook = None
                cast_one(l)
                P.pool_hook = hk
        flush_casts(0)
        P.dma(sp, misem, cst[:], cst_d[:, :], [], [cst_b])
        P.dma(sp, pvsem, pv[:].rearrange("p a l c -> p (a l c)"), pv_d[:, :], [], [pv_b])
        P.op(dve, lambda e: e.tensor_copy(out=identb[:], in_=cst[:, 0:128]), [cst_b], [constb])
        for i, v in enumerate([1.0 / 1024, 1.0 / 4096, 1.0 / 128, 1.0]):
            P.op(dve, lambda e, i=i, v=v: e.memset(ones[:, i, :], v), [], [constb])
        P.op(dve, lambda e: e.memset(hgst[:].rearrange("p l n -> p (l n)"), 0.0), [], [b for bl in hgst_b for b in bl])
        P.op(dve, lambda e: e.memset(lruh[:].rearrange("p l n -> p (l n)"), 0.0), [], [b for bl in lruh_b for b in bl])
        P.op(dve, lambda e: e.memset(xtail[:].rearrange("p l c k -> p (l c k)"), 0.0), [], xtail_b)
        ident = cst[:, 0:128]
        mask_tril = cst[:, 128:256]
        mask_ch = cst[:, 256:768]
        rmask = cst[:, 768:1280]

        def pvs(i):
            return pv[:, i, :, :]

        def dvs(i):
            return dv[:, i, :, :]
        P.op(dve, lambda e: e.tensor_scalar(out=dvs(0), in0=pvs(1), scalar1=0.5, scalar2=None, op0=ALU.mult), [pv_b], [dv_b])
        for k in range(3):
            P.op(dve, lambda e, k=k: e.tensor_scalar(out=dvs(1 + k), in0=pvs(2 + k), scalar1=0.5, scalar2=None, op0=ALU.mult), [pv_b], [dv_b])
        P.op(dve, lambda e: e.tensor_scalar(out=dvs(4), in0=pvs(12), scalar1=0.5, scalar2=None, op0=ALU.mult), [pv_b], [dv_b])
        P.op(dve, lambda e: e.tensor_scalar(out=dvs(5), in0=pvs(13), scalar1=0.5, scalar2=None, op0=ALU.mult), [pv_b], [dv_b])
        P.op(act, lambda e: e.activation(out=ptmp[:, 0], in_=pvs(14), func=AF.Exp, scale=-1.0), [pv_b], [ptmp_b])
        P.op(act, lambda e: e.activation(out=ptmp[:, 1], in_=ptmp[:, 0], func=AF.Ln, bias=1.0, scale=1.0), [ptmp_b], [ptmp_b])
        P.op(dve, lambda e: e.tensor_scalar(out=dvs(6), in0=ptmp[:, 1], scalar1=-4.0, scalar2=None, op0=ALU.mult), [ptmp_b], [dv_b])
        P.op(dve, lambda e: e.tensor_scalar(out=dvs(7), in0=ptmp[:, 1], scalar1=-8.0, scalar2=None, op0=ALU.mult), [ptmp_b], [dv_b])
        lbx = pv[:, 15]
        mx = ptmp[:, 2, 0, :]
        P.op(dve, lambda e: e.tensor_copy(out=mx, in_=lbx[:, 0, :]), [pv_b], [ptmp_b])
        for l in range(1, L):
            P.op(dve, lambda e, l=l: e.tensor_tensor(out=mx, in0=mx, in1=lbx[:, l, :], op=ALU.max), [pv_b, ptmp_b], [ptmp_b])
        for l in range(L):
            P.op(dve, lambda e, l=l: e.tensor_tensor(out=ptmp[:, 3, l, :], in0=lbx[:, l, :], in1=mx, op=ALU.subtract), [pv_b, ptmp_b], [ptmp_b])
        P.op(act, lambda e: e.activation(out=ptmp[:, 4], in_=ptmp[:, 3], func=AF.Exp), [ptmp_b], [ptmp_b])
        sm = ptmp[:, 2, 1 % L if L > 1 else 0, :]
        sm = ptmp[:, 5, 0, :]
        P.op(dve, lambda e: e.tensor_copy(out=sm, in_=ptmp[:, 4, 0, :]), [ptmp_b], [ptmp_b])
        for l in range(1, L):
            P.op(dve, lambda e, l=l: e.tensor_tensor(out=sm, in0=sm, in1=ptmp[:, 4, l, :], op=ALU.add), [ptmp_b], [ptmp_b])
        if L > 1:
            rs = ptmp[:, 5, 1, :]
        else:
            rs = ptmp[:, 2, 0, :]
        P.op(dve, lambda e: e.reciprocal(out=rs, in_=sm), [ptmp_b], [ptmp_b])
        for l in range(L):
            P.op(dve, lambda e, l=l: e.tensor_tensor(out=ptmp[:, 3, l, :], in0=ptmp[:, 4, l, :], in1=rs, op=ALU.mult), [ptmp_b], [ptmp_b])
        P.op(dve, lambda e: e.memset(ptmp[:, 4, 0, :], 0.0), [ptmp_b], [ptmp_b])
        for l in range(1, L):
            P.op(dve, lambda e, l=l: e.tensor_tensor(out=ptmp[:, 4, l, :], in0=ptmp[:, 4, l - 1, :], in1=ptmp[:, 3, l, :], op=ALU.add), [ptmp_b], [ptmp_b])
        P.op(dve, lambda e: e.tensor_scalar(out=dvs(8), in0=ptmp[:, 4], scalar1=-0.5, scalar2=0.5, op0=ALU.mult, op1=ALU.add), [ptmp_b], [dv_b])
        P.op(dve, lambda e: e.tensor_scalar(out=dvs(9), in0=ptmp[:, 4], scalar1=0.5, scalar2=0.5, op0=ALU.mult, op1=ALU.add), [ptmp_b], [dv_b])
        P.op(dve, lambda e: e.tensor_scalar(out=dvs(10), in0=ptmp[:, 4], scalar1=0.5, scalar2=-0.5, op0=ALU.mult, op1=ALU.add), [ptmp_b], [dv_b])

        stream = {"issued": 0, "consumed": 0, "released": 0}
        units = [(b, j, l) for b in range(NSEQ) for j in range(NTILE) for l in range(L)]
        total_groups = len(units) * NG

        def issue_loads(upto):
            while stream["issued"] < min(upto, total_groups):
                k = stream["issued"]
                u, g = divmod(k, NG)
                l = units[u][2]
                flush_casts(l)
                s = k % NSLOT
                P.dma(sp, wsem[s], wring[:, s, 0:GLEN[g]], wb_d[l, g, :, 0:GLEN[g]], [scr_b[l][g]], [wring_b[s]])
                stream["issued"] += 1

        def next_group():
            k = stream["consumed"]
            issue_loads(stream["released"] + NSLOT)
            assert k < stream["issued"], (k, stream)
            stream["consumed"] += 1
            s = k % NSLOT
            return wring[:, s, :].rearrange("p (b k c) -> p b k c", b=4, k=8), wring_b[s], wring[:, s, :]

        def release(n=1):
            stream["released"] += n
            assert stream["released"] <= stream["consumed"]
            issue_loads(stream["released"] + NSLOT)

        def mm_fm(w, wb, blk, rhs_ap, rhs_b, n=T, reserve=False):
            pa, pb = bank(reserve)
            for kc in range(KC):
                P.op(pe, lambda e, kc=kc: e.matmul(pa[:, 0:n], lhsT=w[:, blk, kc, :], rhs=rhs_ap(kc),
                                                   start=(kc == 0), stop=(kc == KC - 1)),
                     [wb, rhs_b[kc]], [pb])
            return pa, pb

        def mm_tm(w, wb, tb, lhs, lhs_b):
            pa, pb = bank()
            for kc in range(KC):
                P.op(pe, lambda e, kc=kc: e.matmul(pa, lhsT=lhs[:, kc, tb * 128:(tb + 1) * 128], rhs=w[:, :, kc, :],
                                                   start=(kc == 0), stop=(kc == KC - 1)),
                     [wb, lhs_b[kc]], [pb])
            return pa, pb

        def rms_rstd(src_ap, src_bufs, ones_idx, nchunks, from_psum=False):
            pa, pb = bank()
            for c in range(nchunks):
                sq, sqb = T16()
                P.op(act, lambda e, c=c, sq=sq: e.activation(out=sq, in_=src_ap(c), func=AF.Square), [src_bufs[c]], [sqb])
                P.op(pe, lambda e, c=c, sq=sq: e.matmul(pa, lhsT=ones[:, ones_idx, :], rhs=sq, start=(c == 0), stop=(c == nchunks - 1)),
                     [sqb, constb], [pb])
            sd, sdb = T32()
            if DEBUG["stage"] == 0.2:
                return sd, sdb
            P.op(act, lambda e: e.activation(out=sd, in_=pa, func=AF.Sqrt, bias=EPS, scale=1.0), [pb], [sdb])
            rstd, rb = T32()
            if DEBUG["stage"] == 0.4:
                return sd, sdb
            P.op(dve, lambda e: e.reciprocal(out=rstd, in_=sd), [sdb], [rb])
            return rstd, rb

        def gate_merge(l, br, ypa, ypb, mpa, mpb, d):
            th, thb = T32()
            P.op(act, lambda e: e.activation(out=th, in_=mpa, func=AF.Tanh, bias=dv[:, 1 + br, l, d:d + 1], scale=0.5), [mpb, dv_b], [thb])
            if DEBUG["stage"] == 1.7:
                return
            if br == DEBUG["branches"].find("abc"[br]) == 0 or "abc"[br] == DEBUG["branches"][0]:
                P.op(dve, lambda e: e.scalar_tensor_tensor(out=merged[:, d, :], in0=th, scalar=1.0, in1=ypa, op0=ALU.add, op1=ALU.mult),
                     [thb, ypb], [merged_b[d]])
            else:
                tm, tmb = T32()
                P.op(dve, lambda e: e.scalar_tensor_tensor(out=tm, in0=th, scalar=1.0, in1=ypa, op0=ALU.add, op1=ALU.mult),
                     [thb, ypb], [tmb])
                P.op(pool, lambda e: e.tensor_tensor(out=merged[:, d, :], in0=merged[:, d, :], in1=tm, op=ALU.add),
                     [tmb, merged_b[d]], [merged_b[d]])

        def proj_and_merge(l, br, yin, yin_b):
            for half in range(2):
                wm, wmb, _ = next_group()
                wp, wpb, _ = next_group()
                for i in range(4):
                    d = half * 4 + i
                    mpa, mpb = mm_fm(wm, wmb, i, lambda kc: xn[:, kc, :], xn_b)
                    ypa, ypb = mm_fm(wp, wpb, i, lambda kc: yin[:, kc, :], yin_b)
                    if "abc"[br] in DEBUG["branches"] and DEBUG["stage"] != 1.6:
                        gate_merge(l, br, ypa, ypb, mpa, mpb, d)
                release(2)

        def unit(l):
            rstd, rb = rms_rstd(lambda c: xres[:, c, :], xres_b, 0, KC)
            if DEBUG["stage"] < 0.8:
                return
            for c in range(KC):
                P.op(dve, lambda e, c=c: e.scalar_tensor_tensor(out=xn[:, c, :], in0=xres[:, c, :], scalar=pv[:, 0, l, c:c + 1], in1=rstd,
                                                                  op0=ALU.mult, op1=ALU.mult),
                     [xres_b[c], rb, pv_b], [xn_b[c]])

            if DEBUG["stage"] < 1:
                return
            nbf, nbf_b = big[0], big_b[0]
            nview = nbf[:].rearrange("p a t -> p (a t)").rearrange("p (tb n) -> p tb n", tb=4)
            wv = [next_group(), next_group()]
            for tb in range(4):
                for half in range(2):
                    dch = 2 * tb + half
                    pa, pb = mm_tm(wv[half][0], wv[half][1], tb, xn, xn_b)
                    P.op(act, lambda e, pa=pa, dch=dch: e.activation(out=merged[:, dch, :], in_=pa, func=AF.Gelu_apprx_tanh),
                         [pb], [merged_b[dch]])
                    P.op(dve, lambda e, half=half, dch=dch: e.bn_stats(out=stat[:, tb, half * 6:(half + 1) * 6], in_=merged[:, dch, :]),
                         [merged_b[dch]], [stat_b[tb]])
                P.op(dve, lambda e: e.bn_aggr(out=stat[:, tb, 12:14], in_=stat[:, tb, 0:12]), [stat_b[tb]], [stat_b[tb]])
            P.op(act, lambda e: e.activation(out=stat[:, :, 14], in_=stat[:, :, 13], func=AF.Sqrt, bias=EPS, scale=1.0), stat_b, stat_b)
            P.op(dve, lambda e: e.reciprocal(out=stat[:, :, 15], in_=stat[:, :, 14]), stat_b, stat_b)
            for tb in range(4):
                P.op(dve, lambda e: e.tensor_scalar(out=nview[:, tb, :], in0=xio_v[:, tb, :], scalar1=stat[:, tb, 12:13], scalar2=stat[:, tb, 15:16],
                                                    op0=ALU.subtract, op1=ALU.mult),
                     [merged_b[2 * tb], merged_b[2 * tb + 1], stat_b[tb]], nbf_b)
            release(2)
            if DEBUG["stage"] in (1.1, 1.2):
                return
            _, auxb, auxflat = next_group()
            wsT = auxflat[:, 0:1024].rearrange("p (g t) -> p g t", g=8)
            P.op(pool, lambda e: e.tensor_tensor(out=wsm[:], in0=wsT, in1=mask_tril.unsqueeze(1).to_broadcast([128, 8, 128]), op=ALU.mult),
                 [auxb, cst_b], [wsm_b])
            release(1)
            if DEBUG["stage"] == 1.3:
                return
            P.dma(sp, m2sem, m2[:].rearrange("p g t -> p (g t)"), bs_d[l:l + 1, :].partition_broadcast(128), [], [m2_b])
            for hf in range(2):
                pa, pb = bank()
                P.op(pe, lambda e, pa=pa, hf=hf: e.matmul(pa, lhsT=ones[:, 3, :], rhs=wsm[:, hf * 4:(hf + 1) * 4, :], start=True, stop=True),
                     [wsm_b, constb], [pb])
                for gg in range(4):
                    g = hf * 4 + gg
                    P.op(dve, lambda e, pa=pa, gg=gg, g=g: e.scalar_tensor_tensor(out=m2[:, g, :], in0=pa[:, gg * 128:(gg + 1) * 128], scalar=pv[:, 6, l, g:g + 1],
                                                                                  in1=m2[:, g, :], op0=ALU.mult, op1=ALU.add),
                         [pb, m2_b, pv_b], [m2_b])
            if DEBUG["stage"] == 1.4:
                return
            yin, yin_b = big[1], big_b[1]
            for half in range(2):
                wu, wub, _ = next_group()
                wg, wgb, _ = next_group()
                for pair in range(2):
                    ii = [pair * 2, pair * 2 + 1]
                    st_ = {}
                    for i in ii:
                        upa, upb = mm_fm(wu, wub, i, lambda kc: xn[:, kc, :], xn_b)
                        ug, ugb = T16()
                        P.op(act, lambda e: e.activation(out=ug, in_=upa, func=AF.Gelu_apprx_tanh), [upb], [ugb])
                        st_[i] = {"ug": (ug, ugb)}
                    for i in ii:
                        st_[i]["g"] = mm_fm(wg, wgb, i, lambda kc: xn[:, kc, :], xn_b)
                    for i in ii:
                        c = half * 4 + i
                        spa, spb = bank()
                        for tb in range(4):
                            P.op(pe, lambda e, tb=tb: e.matmul(spa[:, tb * 128:(tb + 1) * 128], lhsT=nview[:, tb, c * 128:(c + 1) * 128], rhs=wsm[:, c, :],
                                                               start=True, stop=True),
                                 nbf_b + [wsm_b], [spb])
                        st_[i]["sp"] = (spa, spb)
                    for i in ii:
                        gpa, gpb = st_[i]["g"]
                        sg, sgb = T16()
                        P.op(act, lambda e: e.activation(out=sg, in_=gpa, func=AF.Silu), [gpb], [sgb])
                        st_[i]["sg"] = (sg, sgb)
                    for i in ii:
                        c = half * 4 + i
                        spa, spb = st_[i]["sp"]
                        ug, ugb = st_[i]["ug"]
                        sg, sgb = st_[i]["sg"]
                        mx_, mxb = T32()
                        P.op(dve, lambda e: e.scalar_tensor_tensor(out=mx_.rearrange("p (a t) -> p a t", a=4), in0=spa.rearrange("p (a t) -> p a t", a=4),
                                                                   scalar=pv[:, 5, l, c:c + 1],
                                                                   in1=m2[:, c, :].unsqueeze(1).to_broadcast([128, 4, 128]), op0=ALU.mult, op1=ALU.add),
                             [spb, m2_b, pv_b], [mxb])
                        P.op(pool, lambda e: e.tensor_tensor(out=ug, in0=ug, in1=sg, op=ALU.mult), [ugb, sgb], [ugb])
                        P.op(pool, lambda e: e.tensor_tensor(out=yin[:, c, :], in0=mx_, in1=ug, op=ALU.mult), [mxb, ugb], [yin_b[c]])
                release(2)
            if DEBUG["stage"] == 1.5:
                return
            proj_and_merge(l, 0, yin, yin_b)

            if DEBUG["stage"] < 2:
                return
            _, auxb, auxflat = next_group()
            P.op(pool, lambda e: e.tensor_copy(out=wri[:], in_=auxflat[:, 0:2048]), [auxb], [wri_b])
            release(1)
            auxb = wri_b
            wr = wri[:, 0:1024].rearrange("p (h j) -> p h j", h=8)
            wi = wri[:, 1024:2048].rearrange("p (h j) -> p h j", h=8)
            P.op(pool, lambda e: e.tensor_copy(out=xl[:, :, 0:3], in_=xtail[:, l]), [xtail_b[l]], xl_b)
            yin, yin_b = big[2], big_b[2]
            for half in range(2):
                wx, wxb, _ = next_group()
                wg, wgb, _ = next_group()
                for k in range(4):
                    P.op(pool, lambda e, k=k: e.tensor_tensor(out=dg[:, k], in0=ident.unsqueeze(1).to_broadcast([128, 4, 128]),
                                                               in1=pv[:, 7 + k, l, half * 4:half * 4 + 4].unsqueeze(2).to_broadcast([128, 4, 128]), op=ALU.mult),
                         [cst_b, pv_b], [dg_b])
                for pair in range(2):
                    ii = [pair * 2, pair * 2 + 1]
                    cc_ = [half * 4 + i for i in ii]
                    st_ = {}
                    for i, c in zip(ii, cc_):
                        xpa, xpb = mm_fm(wx, wxb, i, lambda kc: xn[:, kc, :], xn_b)
                        P.op(dve, lambda e: e.tensor_copy(out=xl[:, c, 3:3 + T], in_=xpa), [xpb], [xl_b[c]])
                    for i, c in zip(ii, cc_):
                        cpa, cpb = bank()
                        for k in range(4):
                            P.op(pe, lambda e, k=k: e.matmul(cpa, lhsT=dg[:, k, i, :], rhs=xl[:, c, k:k + T], start=(k == 0), stop=(k == 3)),
                                 [dg_b, xl_b[c]], [cpb])
                        xbf, xbfb = T32()
                        P.op(act, lambda e: e.activation(out=xbf, in_=cpa, func=AF.Identity, bias=pv[:, 11, l, c:c + 1], scale=1.0), [cpb, pv_b], [xbfb])
                        xbb, xbbb = T16()
                        P.op(dve, lambda e: e.tensor_scalar(out=xbb, in0=cpa, scalar1=pv[:, 11, l, c:c + 1], scalar2=None, op0=ALU.add), [cpb, pv_b], [xbbb])
                        st_[c] = {"xbf": (xbf, xbfb), "xbb": (xbb, xbbb)}
                    for i, c in zip(ii, cc_):
                        xbb, xbbb = st_[c]["xbb"]
                        rpa, rpb = bank()
                        P.op(pe, lambda e: e.matmul(rpa, lhsT=wr[:, c, :], rhs=xbb, start=True, stop=True), [auxb, xbbb], [rpb])
                        ipa, ipb = bank()
                        P.op(pe, lambda e: e.matmul(ipa, lhsT=wi[:, c, :], rhs=xbb, start=True, stop=True), [auxb, xbbb], [ipb])
                        st_[c]["r"] = (rpa, rpb)
                        st_[c]["i"] = (ipa, ipb)
                    for i, c in zip(ii, cc_):
                        rpa, rpb = st_[c]["r"]
                        ipa, ipb = st_[c]["i"]
                        thr, thrb = T32()
                        P.op(act, lambda e: e.activation(out=thr, in_=rpa, func=AF.Tanh, bias=dv[:, 4, l, c:c + 1], scale=0.5), [rpb, dv_b], [thrb])
                        thi, thib = T32()
                        P.op(act, lambda e: e.activation(out=thi, in_=ipa, func=AF.Tanh, bias=dv[:, 5, l, c:c + 1], scale=0.5), [ipb, dv_b], [thib])
                        st_[c]["thr"] = (thr, thrb)
                        st_[c]["thi"] = (thi, thib)
                    for i, c in zip(ii, cc_):
                        st_[c]["g"] = mm_fm(wg, wgb, i, lambda kc: xn[:, kc, :], xn_b)
                    for i, c in zip(ii, cc_):
                        thr, thrb = st_[c]["thr"]
                        a2, a2b = T32()
                        P.op(act, lambda e: e.activation(out=thr, in_=thr, func=AF.Exp, bias=dv[:, 6, l, c:c + 1], scale=dv[:, 6, l, c:c + 1]), [thrb, dv_b], [thrb])
                        P.op(pool, lambda e: e.tensor_tensor(out=a2, in0=thr, in1=thr, op=ALU.mult), [thrb], [a2b])
                        st_[c]["a2"] = (a2, a2b)
                    for i, c in zip(ii, cc_):
                        a2, a2b = st_[c]["a2"]
                        P.op(act, lambda e: e.activation(out=a2, in_=a2, func=AF.Sqrt, bias=0.25, scale=-0.25), [a2b], [a2b])
                    for i, c in zip(ii, cc_):
                        gpa, gpb = st_[c]["g"]
                        sg, sgb = T16()
                        P.op(act, lambda e: e.activation(out=sg, in_=gpa, func=AF.Silu), [gpb], [sgb])
                        st_[c]["sg"] = (sg, sgb)
                    for i, c in zip(ii, cc_):
                        xbf, xbfb = st_[c]["xbf"]
                        thi, thib = st_[c]["thi"]
                        aa, aab = st_[c]["thr"]
                        a2, a2b = st_[c]["a2"]
                        sg, sgb = st_[c]["sg"]
                        P.op(dve, lambda e: e.scalar_tensor_tensor(out=thi, in0=thi, scalar=1.0, in1=xbf, op0=ALU.add, op1=ALU.mult), [thib, xbfb], [thib])
                        P.op(pool, lambda e: e.tensor_tensor(out=thi, in0=thi, in1=a2, op=ALU.mult), [thib, a2b], [thib])
                        P.op(dve, lambda e: e.tensor_tensor_scan(out=xbf, data0=aa, data1=thi, initial=lruh[:, l, c:c + 1], op0=ALU.mult, op1=ALU.add),
                             [aab, thib, lruh_b[l][c]], [xbfb])
                        P.op(pool, lambda e: e.tensor_copy(out=lruh[:, l, c:c + 1], in_=xbf[:, T - 1:T]), [xbfb], [lruh_b[l][c]])
                        P.op(pool, lambda e: e.tensor_tensor(out=yin[:, c, :], in0=xbf, in1=sg, op=ALU.mult), [xbfb, sgb], [yin_b[c]])
                release(2)
            P.op(pool, lambda e: e.tensor_copy(out=xtail[:, l], in_=xl[:, :, T:T + 3]), xl_b, [xtail_b[l]])
            proj_and_merge(l, 1, yin, yin_b)

            if DEBUG["stage"] < 3:
                return
            vt_, vt_b = big[0], big_b[0]
            vview = vt_[:].rearrange("p a t -> p (a t)").rearrange("p (tb n) -> p tb n", tb=4)
            wv = [next_group(), next_group()]
            for tb in range(4):
                for half in range(2):
                    pa, pb = mm_tm(wv[half][0], wv[half][1], tb, xn, xn_b)
                    P.op(act, lambda e, pa=pa, half=half, tb=tb: e.activation(out=vview[:, tb, half * 512:(half + 1) * 512], in_=pa, func=AF.Copy),
                         [pb], vt_b)
            release(2)
            yin, yin_b = big[1], big_b[1]
            qg_, qg_b = big[2], big_b[2]
            kd_, kd_b = big[3], big_b[3]
            for hg in range(2):
                wq, wqb, _ = next_group()
                wf, wfb, _ = next_group()
                for pair in range(2):
                    ii = [pair * 2, pair * 2 + 1]
                    st_ = {}
                    for i in ii:
                        fpa, fpb = mm_fm(wf, wfb, i, lambda kc: xn[:, kc, :], xn_b)
                        th, thb = T32()
                        P.op(act, lambda e: e.activation(out=th, in_=fpa, func=AF.Tanh, scale=0.5), [fpb], [thb])
                        st_[i] = {"th": (th, thb)}
                    for i in ii:
                        st_[i]["q"] = mm_fm(wq, wqb, i, lambda kc: xn[:, kc, :], xn_b)
                    for i in ii:
                        h = hg * 4 + i
                        th, thb = st_[i]["th"]
                        ff, ffb = T32()
                        P.op(dve, lambda e: e.tensor_scalar(out=ff, in0=th, scalar1=dv[:, 8, l, h:h + 1], scalar2=dv[:, 9, l, h:h + 1], op0=ALU.mult, op1=ALU.add),
                             [thb, dv_b], [ffb])
                        G, Gb = T32()
                        P.op(dve, lambda e: e.tensor_tensor_scan(out=G, data0=rmask, data1=ff, initial=1.0, op0=ALU.max, op1=ALU.mult), [ffb, cst_b], [Gb])
                        P.op(pool, lambda e: e.tensor_copy(out=glast[:, i, :], in_=G.rearrange("p (c t) -> p c t", t=32)[:, :, 31]), [Gb], [glast_b[i]])
                        rG = ff
                        P.op(dve, lambda e: e.reciprocal(out=rG, in_=G), [Gb, ffb], [ffb])
                        P.op(pool, lambda e: e.tensor_scalar(out=th, in0=th, scalar1=dv[:, 10, l, h:h + 1], scalar2=dv[:, 8, l, h:h + 1], op0=ALU.mult, op1=ALU.add),
                             [thb, dv_b], [thb])
                        kg, kgb = T16()
                        P.op(pool, lambda e: e.tensor_tensor(out=kg, in0=th, in1=rG, op=ALU.mult), [thb, ffb], [kgb])
                        kdec, kdecb = T16()
                        P.op(pool, lambda e: e.tensor_tensor(out=kdec.rearrange("p (c t) -> p c t", t=32), in0=kg.rearrange("p (c t) -> p c t", t=32),
                                                             in1=glast[:, i, :].unsqueeze(2).to_broadcast([128, 16, 32]), op=ALU.mult),
                             [kgb, glast_b[i]], [kdecb])
                        qpa, qpb = st_[i]["q"]
                        P.op(dve, lambda e: e.tensor_tensor(out=qg_[:, i, :], in0=qpa, in1=G, op=ALU.mult), [qpb, Gb], [qg_b[i]])
                        st_[i]["kg"] = (kg, kgb)
                        st_[i]["kdec"] = (kdec, kdecb)
                    for i in ii:
                        kg, kgb = st_[i]["kg"]
                        kdec, kdecb = st_[i]["kdec"]
                        spa, spb = bank()
                        for tb in range(4):
                            P.op(pe, lambda e, tb=tb: e.matmul(spa[:, tb * 128:(tb + 1) * 128], lhsT=kg[:, tb * 128:(tb + 1) * 128],
                                                               rhs=qg_[:, i, tb * 128:(tb + 1) * 128], start=True, stop=True),
                                 [kgb, qg_b[i]], [spb])
                        P.op(dve, lambda e: e.tensor_tensor(out=kd_[:, 4 + i, :], in0=spa, in1=mask_ch, op=ALU.mult), [spb, cst_b], [kd_b[4 + i]])
                        tpa, tpb = bank()
                        tpbf = tpa.bitcast(BF16)
                        for tb in range(4):
                            P.op(pe, lambda e, tb=tb: e.transpose(tpbf[:, tb * 128:(tb + 1) * 128], kdec[:, tb * 128:(tb + 1) * 128], identb[:]),
                                 [kdecb, constb], [tpb])
                        P.op(act, lambda e: e.activation(out=kd_[:, i, :], in_=tpbf[:, 0:512], func=AF.Copy), [tpb], [kd_b[i]])
                release(2)
                wgc, wgcb, _ = next_group()
                for i in range(4):
                    h = hg * 4 + i
                    P.op(act, lambda e, i=i, h=h: e.activation(out=sall[:, 0, i * 128:(i + 1) * 128], in_=hgst[:, l, h * 128:(h + 1) * 128], func=AF.Copy),
                         [hgst_b[l][h]], [sall_b[0]])
                sgs = {}
                for c in range(16):
                    tb, cc = divmod(c, 4)
                    upa, upb = bank()
                    for i in range(4):
                        h = hg * 4 + i
                        P.op(pe, lambda e, i=i, h=h: e.matmul(upa[:, i * 128:(i + 1) * 128], lhsT=kd_[cc * 32:(cc + 1) * 32, i, tb * 128:(tb + 1) * 128],
                                                             rhs=vview[cc * 32:(cc + 1) * 32, tb, h * 128:(h + 1) * 128], start=True, stop=True,
                                                             tile_position=(cc * 32, 0)),
                             [kd_b[i]] + vt_b, [upb])
                    for i in range(4):
                        h = hg * 4 + i
                        if c == 0:
                            src, srcb = hgst[:, l, h * 128:(h + 1) * 128], hgst_b[l][h]
                        else:
                            src, srcb = stw[:, c % 2, i * 128:(i + 1) * 128], stw_b[c % 2][i]
                        if c == 15:
                            dst, dstb = hgst[:, l, h * 128:(h + 1) * 128], hgst_b[l][h]
                        else:
                            dst, dstb = stw[:, (c + 1) % 2, i * 128:(i + 1) * 128], stw_b[(c + 1) % 2][i]
                        P.op(dve, lambda e, i=i, src=src, dst=dst: e.scalar_tensor_tensor(out=dst, in0=src, scalar=glast[:, i, c:c + 1], in1=upa[:, i * 128:(i + 1) * 128],
                                                                                         op0=ALU.mult, op1=ALU.add),
                             [srcb, glast_b[i], upb], [dstb])
                        if c < 15:
                            P.op(act, lambda e, i=i, dst=dst: e.activation(out=sall[:, c + 1, i * 128:(i + 1) * 128], in_=dst, func=AF.Copy),
                                 [dstb], [sall_b[c + 1]])
                    if c % 4 == 3:
                        i = c // 4
                        gpa, gpb = mm_fm(wgc, wgcb, i, lambda kc: xn[:, kc, :], xn_b)
                        P.op(act, lambda e, i=i, gpa=gpa: e.activation(out=sgc[:, i, :], in_=gpa, func=AF.Silu), [gpb], [sgc_b[i]])
                release(1)
                for i in range(4):
                    h = hg * 4 + i
                    opa, opb = bank()
                    for tb in range(4):
                        P.op(pe, lambda e, tb=tb, h=h, i=i: e.matmul(opa[:, tb * 128:(tb + 1) * 128], lhsT=vview[:, tb, h * 128:(h + 1) * 128],
                                                                     rhs=kd_[:, 4 + i, tb * 128:(tb + 1) * 128], start=True, stop=False),
                             vt_b + [kd_b[4 + i]], [opb])
                        for cc in range(4):
                            c = tb * 4 + cc
                            P.op(pe, lambda e, c=c, i=i, cc=cc: e.matmul(opa[:, c * 32:(c + 1) * 32], lhsT=sall[:, c, i * 128:(i + 1) * 128],
                                                                         rhs=qg_[:, i, c * 32:(c + 1) * 32], start=False, stop=(cc == 3)),
                                 [sall_b[c], qg_b[i]], [opb])
                    rstd, rb = rms_rstd(lambda c_: opa, [opb], 2, 1)
                    on, onb = T32()
                    P.op(dve, lambda e: e.scalar_tensor_tensor(out=on, in0=opa, scalar=pv[:, 16, l, h:h + 1], in1=rstd, op0=ALU.mult, op1=ALU.mult),
                         [opb, rb, pv_b], [onb])
                    P.op(pool, lambda e: e.tensor_tensor(out=yin[:, h, :], in0=on, in1=sgc[:, i, :], op=ALU.mult), [onb, sgc_b[i]], [yin_b[h]])
            proj_and_merge(l, 2, yin, yin_b)

            if DEBUG["stage"] < 4:
                return
            mbf, mbf_b = big[2], big_b[2]
            for c in range(KC):
                P.op(act, lambda e, c=c: e.activation(out=mbf[:, c, :], in_=merged[:, c, :], func=AF.Copy), [merged_b[c]], [mbf_b[c]])
            ppa, ppb = bank(reserve=True)
            for half in range(2):
                wo, wob, _ = next_group()
                for i in range(4):
                    d = half * 4 + i
                    ypa, ypb = mm_fm(wo, wob, i, lambda kc: mbf[:, kc, :], mbf_b)
                    sq, sqb = T16()
                    P.op(act, lambda e: e.activation(out=sq, in_=ypa, func=AF.Square), [ypb], [sqb])
                    P.op(pe, lambda e, d=d, sq=sq: e.matmul(ppa, lhsT=ones[:, 1, :], rhs=sq, start=(d == 0), stop=(d == 7)), [sqb, constb], [ppb])
                    P.op(dve, lambda e, d=d: e.tensor_scalar(out=merged[:, d, :], in0=ypa, scalar1=dv[:, 0, l, d:d + 1], scalar2=None, op0=ALU.mult),
                         [ypb, dv_b], [merged_b[d]])
                release(1)
            resv.clear()
            sd, sdb = T32()
            P.op(act, lambda e: e.activation(out=sd, in_=ppa, func=AF.Sqrt, bias=EPS, scale=1.0), [ppb], [sdb])
            rstd, rb = T32()
            P.op(dve, lambda e: e.reciprocal(out=rstd, in_=sd), [sdb], [rb])
            for d in range(KC):
                P.op(dve, lambda e, d=d: e.tensor_tensor(out=merged[:, d, :], in0=merged[:, d, :], in1=rstd, op=ALU.mult), [merged_b[d], rb], [merged_b[d]])
                P.op(pool, lambda e, d=d: e.tensor_tensor(out=xres[:, d, :], in0=xres[:, d, :], in1=merged[:, d, :], op=ALU.add),
                     [merged_b[d], xres_b[d]], [xres_b[d]])

        xio = merged[:].rearrange("p a t -> p (a t)").rearrange("p (tb n) -> p tb n", tb=4)
        for b in range(NSEQ):
            if b > 0:
                P.op(dve, lambda e: e.memset(hgst[:].rearrange("p l n -> p (l n)"), 0.0), [], [bb for bl in hgst_b for bb in bl])
                P.op(dve, lambda e: e.memset(lruh[:].rearrange("p l n -> p (l n)"), 0.0), [], [bb for bl in lruh_b for bb in bl])
                P.op(dve, lambda e: e.memset(xtail[:].rearrange("p l c k -> p (l c k)"), 0.0), [], xtail_b)
            for j in range(NTILE):
                P.dma(sp, iosem, xio, x_d[b, j * T:(j + 1) * T, :].rearrange("(tb p) n -> p tb n", p=128), [], merged_b)
                for c in range(KC):
                    pa, pb = bank()
                    for tb in range(4):
                        P.op(pe, lambda e, tb=tb, c=c: e.transpose(pa[:, tb * 128:(tb + 1) * 128], xio[:, tb, c * 128:(c + 1) * 128], ident),
                             merged_b + [cst_b], [pb])
                    P.op(act, lambda e, c=c, pa=pa: e.activation(out=xres[:, c, :], in_=pa, func=AF.Copy), [pb], [xres_b[c]])
                for l in range(L):
                    if b == 0 and j == 0:
                        hook_state["layer"] = l + 1
                        hook_state["n"] = 0
                        P.pool_hook = pool_hook
                    unit(l)
                    P.pool_hook = None
                for tb in range(4):
                    for hf in range(2):
                        pa, pb = bank()
                        for cc in range(4):
                            c = hf * 4 + cc
                            P.op(pe, lambda e, cc=cc, c=c, tb=tb: e.transpose(pa[:, cc * 128:(cc + 1) * 128], xres[:, c, tb * 128:(tb + 1) * 128], ident),
                                 [xres_b[c], cst_b], [pb])
                        P.op(act, lambda e, pa=pa, tb=tb, hf=hf: e.activation(out=xio[:, tb, hf * 512:(hf + 1) * 512], in_=pa, func=AF.Copy), [pb], merged_b)
                P.dma(sp, iosem, out_d[b, j * T:(j + 1) * T, :].rearrange("(tb p) n -> p tb n", p=128), xio, merged_b, [])
        if P.dry:
            return P.waited
        nc.gpsimd.wait_ge(iosem[0], iosem[1])
        stats = {e.name: (e.n, e.ninc) for e in (pe, act, dve, pool)}
        print("instr counts (ops, sem incs)", stats, "groups", stream)
    return nc


def _colblock(w, b0):
    sub = w[:, b0 * 128:(b0 + 4) * 128]
    return sub.reshape(8, 128, 4, 128).transpose(1, 2, 0, 3)


def make_consts():
    cst = np.zeros((128, 1280), np.float32)
    cst[:, 0:128] = np.eye(128, dtype=np.float32)
    s = np.arange(128)[:, None]
    t = np.arange(128)[None, :]
    cst[:, 128:256] = (s <= t).astype(np.float32)
    mch = ((s // 32 == t // 32) & (s <= t)).astype(np.float32)
    cst[:, 256:768] = np.tile(mch, (1, 4))
    rm = np.zeros(512, np.float32)
    rm[::32] = 1.0
    cst[:, 768:1280] = rm[None, :]
    return cst


def prep_shared(inp, L):
    ws = np.empty((L, NG, 128, GSZ), np.float32)
    proj = {"a": inp["w_a_proj"], "b": inp["w_b_proj"], "c": inp["w_c_proj"], "o": inp["w_out"]}
    for l in range(L):
        for g, (kind, b0) in enumerate(GROUPS):
            if kind == "in":
                ws[l, g] = _colblock(inp["w_in"][l], b0).reshape(128, GSZ)
            elif kind == "auxA":
                ws[l, g] = 0.0
                ws[l, g, :, 0:1024] = inp["gm_w_s"][l].transpose(2, 0, 1).reshape(128, 1024)
            elif kind == "auxB":
                ws[l, g] = 0.0
                ws[l, g, :, 0:1024] = inp["lru_w_r"][l].transpose(1, 0, 2).reshape(128, 1024)
                ws[l, g, :, 1024:2048] = inp["lru_w_i"][l].transpose(1, 0, 2).reshape(128, 1024)
            else:
                ws[l, g] = _colblock(proj[kind][l], b0).reshape(128, GSZ)
    names = [("pre_norm_g", None), ("post_norm_g", None), ("b_merge", 0), ("b_merge", 1), ("b_merge", 2),
             ("gm_ln_g", None), ("gm_ln_b", None), ("lru_conv_w", 0), ("lru_conv_w", 1), ("lru_conv_w", 2), ("lru_conv_w", 3),
             ("lru_conv_b", None), ("lru_b_r", None), ("lru_b_i", None), ("lru_lambda", None), ("hg_lower_bounds", None), ("hg_norm_g", None)]
    pvec = np.empty((128, NPV, L, 8), np.float32)
    for i, (n, k) in enumerate(names):
        a = inp[n][:L] if k is None else inp[n][:L, k]
        pvec[:, i] = a.reshape(L, 8, 128).transpose(2, 0, 1)
    bsrow = np.ascontiguousarray(inp["gm_b_s"][:L].reshape(L, 1024))
    return {"wstream": ws, "pvec": pvec.reshape(128, -1), "bsrow": bsrow, "cst": make_consts()}


def run(inputs, L, n_cores, nseq_per_core, ntile):
    inp = {k: np.asarray(v) for k, v in inputs.items()}
    shared = prep_shared(inp, L)
    need = build_program(L, nseq_per_core, ntile, None)
    nc = build_program(L, nseq_per_core, ntile, need)
    x = np.ascontiguousarray(inp["x"], dtype=np.float32)
    in_maps = []
    for c in range(n_cores):
        m = dict(shared)
        m["x"] = np.ascontiguousarray(x[c * nseq_per_core:(c + 1) * nseq_per_core])
        in_maps.append(m)
    res = run_bass_kernel_spmd(nc, in_maps, core_ids=list(range(n_cores)))
    return np.concatenate([r["out"] for r in res.results], axis=0)


def kernel(**inputs):
    return run(inputs, 4, 8, 4, 4)
```

```python
import numpy as np
import concourse.bass as bass
import concourse.mybir as mybir
from concourse.bass_utils import run_bass_kernel_spmd

F32 = mybir.dt.float32
BF16 = mybir.dt.bfloat16
AF = mybir.ActivationFunctionType
ALU = mybir.AluOpType

D = 1024
KC = 8
T = 512
INC = 12288
NG = 34
GSZ = 4096
EPS = 1e-6
NPV = 17
NDV = 11
NSLOT = 4

CB_U, CB_V, CB_GA, CB_X, CB_GB, CB_Q, CB_F, CB_I, CB_GC, CB_M = 0, 8, 16, 24, 32, 40, 48, 56, 64, 72

GROUPS = [
    ("in", CB_V), ("in", CB_V + 4), ("auxA", 0),
    ("in", CB_U), ("in", CB_GA), ("in", CB_U + 4), ("in", CB_GA + 4),
    ("in", CB_M), ("a", 0), ("in", CB_M + 4), ("a", 4),
    ("auxB", 0), ("in", CB_X), ("in", CB_GB), ("in", CB_X + 4), ("in", CB_GB + 4),
    ("in", CB_M + 8), ("b", 0), ("in", CB_M + 12), ("b", 4),
    ("in", CB_I), ("in", CB_I + 4),
    ("in", CB_Q), ("in", CB_F), ("in", CB_GC), ("in", CB_Q + 4), ("in", CB_F + 4), ("in", CB_GC + 4),
    ("in", CB_M + 16), ("c", 0), ("in", CB_M + 20), ("c", 4),
    ("o", 0), ("o", 4),
]
assert len(GROUPS) == NG
GLEN = [1024 if k == 'auxA' else (2048 if k == 'auxB' else GSZ) for k, _ in GROUPS]


class Buf:
    __slots__ = ("w", "r", "excl")

    def __init__(self, excl=False):
        self.w = None
        self.r = {}
        self.excl = excl


class Eng:
    def __init__(self, name, h, sem, pe=False):
        self.name = name
        self.h = h
        self.sem = sem
        self.n = 0
        self.ninc = 0
        self.comp = {}
        self.seen = {}
        self.pe = pe


class Prog:
    def __init__(self, nc, need):
        self.nc = nc
        self.dry = need is None
        self.need = need
        self.waited = set()
        self.engs = {}
        self.dsems = {}
        self.pool_hook = None

    def _waits(self, eng, reads, writes):
        deps = {}

        def add(d):
            if d is None:
                return
            k = d[0]
            if k not in deps or deps[k] < d[1]:
                deps[k] = d[1]
        for b in reads:
            add(b.w)
        for b in writes:
            add(b.w)
            for d in b.r.items():
                add(d)
        for k, val in deps.items():
            if eng.pe and k == eng.name:
                continue
            if eng.seen.get(k, 0) >= val:
                continue
            eng.seen[k] = val
            if self.dry:
                self.waited.add((k, val))
                continue
            if k in self.engs:
                src = self.engs[k]
                eng.h.wait_ge(src.sem, src.comp[val])
            else:
                eng.h.wait_ge(self.dsems[k], val)

    def _record(self, me, reads, writes):
        for b in reads:
            b.r[me[0]] = me[1]
        for b in writes:
            b.w = me
            b.r = {}

    def op(self, eng, fn, reads, writes):
        ex = [b for b in reads if b.excl]
        if ex:
            reads = [b for b in reads if not b.excl]
            writes = list(writes) + ex
        self._waits(eng, reads, writes)
        eng.n += 1
        if not self.dry:
            ins = fn(eng.h)
            if (eng.name, eng.n) in self.need:
                eng.ninc += 1
                eng.comp[eng.n] = eng.ninc
                ins.then_inc(eng.sem, 1)
        self._record((eng.name, eng.n), reads, writes)
        if self.pool_hook is not None and eng.name == "pool":
            self.pool_hook()

    def dma(self, eng, semc, out, in_, reads, writes):
        self._waits(eng, reads, writes)
        semc[1] += 16
        if not self.dry:
            ins = eng.h.dma_start(out=out, in_=in_)
            ins.then_inc(semc[0], 16)
        self._record((semc[2], semc[1]), reads, writes)


DEBUG = {"branches": "abc", "stage": 9}


def build_program(L, NSEQ, NTILE, need=None):
    nc = bass.Bass("TRN2", target_bir_lowering=False)
    S = NTILE * T
    x_d = nc.dram_tensor("x", [NSEQ, S, D], F32, kind="ExternalInput").ap()
    ws_d = nc.dram_tensor("wstream", [L, NG, 128, GSZ], F32, kind="ExternalInput").ap()
    pv_d = nc.dram_tensor("pvec", [128, NPV * L * 8], F32, kind="ExternalInput").ap()
    bs_d = nc.dram_tensor("bsrow", [L, 1024], F32, kind="ExternalInput").ap()
    cst_d = nc.dram_tensor("cst", [128, 1280], F32, kind="ExternalInput").ap()
    out_d = nc.dram_tensor("out", [NSEQ, S, D], F32, kind="ExternalOutput").ap()
    wb_d = nc.dram_tensor("wbf", [L, NG, 128, GSZ], BF16, kind="Internal").ap()

    import contextlib
    es = contextlib.ExitStack()
    with es:
        def sb(name, shape, dt):
            return es.enter_context(nc.sbuf_tensor("sb_" + name, shape, dt))

        P = Prog(nc, need)

        def sem(name):
            return es.enter_context(nc.semaphore(name))

        def dsem(name):
            h = sem(name)
            P.dsems[name] = h
            return [h, 0, name]

        pe = Eng("pe", nc.tensor, sem("s_pe"), pe=True)
        act = Eng("act", nc.scalar, sem("s_act"))
        dve = Eng("dve", nc.vector, sem("s_dve"))
        pool = Eng("pool", nc.gpsimd, sem("s_pool"))
        sp = Eng("sp", nc.sync, sem("s_sp"))
        for e_ in (pe, act, dve, pool, sp):
            P.engs[e_.name] = e_

        xres = sb("xres", [128, KC, T], F32)
        xres_b = [Buf() for _ in range(KC)]
        merged = sb("merged", [128, KC, T], F32)
        merged_b = [Buf() for _ in range(KC)]
        hgst = sb("hgst", [128, L, 8 * 128], F32)
        hgst_b = [[Buf() for _ in range(8)] for _ in range(L)]
        stw = sb("stw", [128, 2, 4 * 128], F32)
        stw_b = [[Buf() for _ in range(4)] for _ in range(2)]
        lruh = sb("lruh", [128, L, 8], F32)
        lruh_b = [[Buf() for _ in range(8)] for _ in range(L)]
        xl = sb("xl", [128, KC, 3 + T], BF16)
        xl_b = [Buf() for _ in range(KC)]
        xtail = sb("xtail", [128, L, KC, 3], BF16)
        xtail_b = [Buf() for _ in range(L)]
        cst = sb("cst", [128, 1280], F32)
        cst_b = Buf()
        pv = sb("pv", [128, NPV, L, 8], F32)
        pv_b = Buf()
        dv = sb("dv", [128, NDV, L, 8], F32)
        dv_b = Buf()
        ptmp = sb("ptmp", [128, 6, L, 8], F32)
        ptmp_b = Buf()
        identb = sb("identb", [128, 128], BF16)
        ones = sb("ones", [128, 4, 128], BF16)
        constb = Buf()
        wring = sb("wring", [128, NSLOT, GSZ], BF16)
        wring_b = [Buf() for _ in range(NSLOT)]
        wsem = [dsem("w%d" % i) for i in range(NSLOT)]
        xn = sb("xn", [128, KC, T], BF16)
        xn_b = [Buf() for _ in range(KC)]
        NBIG = 4
        big = [sb("big%d" % i, [128, KC, T], BF16) for i in range(NBIG)]
        big_b = [[Buf() for _ in range(KC)] for _ in range(NBIG)]
        sall = sb("sall", [128, 16, 4 * 128], BF16)
        sall_b = [Buf() for _ in range(16)]
        NT32 = 10
        t32 = sb("t32", [128, NT32, T], F32)
        t32_b = [Buf() for _ in range(NT32)]
        NT16 = 6
        t16 = sb("t16", [128, NT16, T], BF16)
        t16_b = [Buf() for _ in range(NT16)]
        sgc = sb("sgc", [128, 4, T], BF16)
        sgc_b = [Buf() for _ in range(4)]
        m2 = sb("m2", [128, 8, 128], F32)
        m2_b = Buf()
        wsm = sb("wsm", [128, 8, 128], BF16)
        wsm_b = Buf()
        wri = sb("wri", [128, 2048], BF16)
        wri_b = Buf()
        dg = sb("dg", [128, 4, 4, 128], BF16)
        dg_b = Buf()
        stat = sb("stat", [128, 4, 16], F32)
        stat_b = [Buf() for _ in range(4)]
        glast = sb("glast", [128, 4, 16], F32)
        glast_b = [Buf() for _ in range(4)]
        ps = es.enter_context(nc.psum_tensor("ps", [128, 8 * 512], F32))
        ps_b = [Buf(excl=True) for _ in range(8)]
        scr_b = [[Buf() for _ in range(NG)] if l == 0 else [Buf()] * NG for l in range(L)]
        cvsem = [[dsem("cv0_%d" % g) for g in range(NG)] if l == 0 else [dsem("cv%d" % l)] * NG for l in range(L)]
        iosem = dsem("io")
        misem = dsem("misc")
        pvsem = dsem("pvs")
        m2sem = dsem("m2s")

        ctr = {"ps": 0, "t32": 0, "t16": 0, "vt": 0}
        xio_v = merged[:].rearrange("p a t -> p (a t)").rearrange("p (tb n) -> p tb n", tb=4)

        resv = set()

        def bank(reserve=False):
            while True:
                i = ctr["ps"] % 8
                ctr["ps"] += 1
                if i not in resv:
                    break
            if reserve:
                resv.add(i)
            return ps[:, i * 512:(i + 1) * 512], ps_b[i]

        def T32():
            i = ctr["t32"] % NT32
            ctr["t32"] += 1
            return t32[:, i, :], t32_b[i]

        def T16():
            i = ctr["t16"] % NT16
            ctr["t16"] += 1
            return t16[:, i, :], t16_b[i]

        pending = {l: list(range(NG)) for l in range(L)}
        hook_state = {"layer": None, "n": 0}

        def cast_one(l):
            g = pending[l].pop(0)
            P.dma(pool, cvsem[l][g], wb_d[l, g, :, 0:GLEN[g]], ws_d[l, g, :, 0:GLEN[g]], [], [scr_b[l][g]])

        def flush_casts(l):
            while pending[l]:
                cast_one(l)

        def pool_hook():
            l = hook_state["layer"]
            if l is None or l >= L or not pending[l]:
                return
            hook_state["n"] += 1
            if hook_state["n"] % 3 == 0:
                hk = P.pool_hook
                P.pool_hook = None
                cast_one(l)
                P.pool_hook = hk
        flush_casts(0)
        P.dma(sp, misem, cst[:], cst_d[:, :], [], [cst_b])
        P.dma(sp, pvsem, pv[:].rearrange("p a l c -> p (a l c)"), pv_d[:, :], [], [pv_b])
        P.op(dve, lambda e: e.tensor_copy(out=identb[:], in_=cst[:, 0:128]), [cst_b], [constb])
        for i, v in enumerate([1.0 / 1024, 1.0 / 4096, 1.0 / 128, 1.0]):
            P.op(dve, lambda e, i=i, v=v: e.memset(ones[:, i, :], v), [], [constb])
        P.op(dve, lambda e: e.memset(hgst[:].rearrange("p l n -> p (l n)"), 0.0), [], [b for bl in hgst_b for b in bl])
        P.op(dve, lambda e: e.memset(lruh[:].rearrange("p l n -> p (l n)"), 0.0), [], [b for bl in lruh_b for b in bl])
        P.op(dve, lambda e: e.memset(xtail[:].rearrange("p l c k -> p (l c k)"), 0.0), [], xtail_b)
        ident = cst[:, 0:128]
        mask_tril = cst[:, 128:256]
        mask_ch = cst[:, 256:768]
        rmask = cst[:, 768:1280]

        def pvs(i):
            return pv[:, i, :, :]

        def dvs(i):
            return dv[:, i, :, :]
        P.op(dve, lambda e: e.tensor_scalar(out=dvs(0), in0=pvs(1), scalar1=0.5, scalar2=None, op0=ALU.mult), [pv_b], [dv_b])
        for k in range(3):
            P.op(dve, lambda e, k=k: e.tensor_scalar(out=dvs(1 + k), in0=pvs(2 + k), scalar1=0.5, scalar2=None, op0=ALU.mult), [pv_b], [dv_b])
        P.op(dve, lambda e: e.tensor_scalar(out=dvs(4), in0=pvs(12), scalar1=0.5, scalar2=None, op0=ALU.mult), [pv_b], [dv_b])
        P.op(dve, lambda e: e.tensor_scalar(out=dvs(5), in0=pvs(13), scalar1=0.5, scalar2=None, op0=ALU.mult), [pv_b], [dv_b])
        P.op(act, lambda e: e.activation(out=ptmp[:, 0], in_=pvs(14), func=AF.Exp, scale=-1.0), [pv_b], [ptmp_b])
        P.op(act, lambda e: e.activation(out=ptmp[:, 1], in_=ptmp[:, 0], func=AF.Ln, bias=1.0, scale=1.0), [ptmp_b], [ptmp_b])
        P.op(dve, lambda e: e.tensor_scalar(out=dvs(6), in0=ptmp[:, 1], scalar1=-4.0, scalar2=None, op0=ALU.mult), [ptmp_b], [dv_b])
        P.op(dve, lambda e: e.tensor_scalar(out=dvs(7), in0=ptmp[:, 1], scalar1=-8.0, scalar2=None, op0=ALU.mult), [ptmp_b], [dv_b])
        lbx = pv[:, 15]
        mx = ptmp[:, 2, 0, :]
        P.op(dve, lambda e: e.tensor_copy(out=mx, in_=lbx[:, 0, :]), [pv_b], [ptmp_b])
        for l in range(1, L):
            P.op(dve, lambda e, l=l: e.tensor_tensor(out=mx, in0=mx, in1=lbx[:, l, :], op=ALU.max), [pv_b, ptmp_b], [ptmp_b])
        for l in range(L):
            P.op(dve, lambda e, l=l: e.tensor_tensor(out=ptmp[:, 3, l, :], in0=lbx[:, l, :], in1=mx, op=ALU.subtract), [pv_b, ptmp_b], [ptmp_b])
        P.op(act, lambda e: e.activation(out=ptmp[:, 4], in_=ptmp[:, 3], func=AF.Exp), [ptmp_b], [ptmp_b])
        sm = ptmp[:, 2, 1 % L if L > 1 else 0, :]
        sm = ptmp[:, 5, 0, :]
        P.op(dve, lambda e: e.tensor_copy(out=sm, in_=ptmp[:, 4, 0, :]), [ptmp_b], [ptmp_b])
        for l in range(1, L):
            P.op(dve, lambda e, l=l: e.tensor_tensor(out=sm, in0=sm, in1=ptmp[:, 4, l, :], op=ALU.add), [ptmp_b], [ptmp_b])
        if L > 1:
            rs = ptmp[:, 5, 1, :]
        else:
            rs = ptmp[:, 2, 0, :]
        P.op(dve, lambda e: e.reciprocal(out=rs, in_=sm), [ptmp_b], [ptmp_b])
        for l in range(L):
            P.op(dve, lambda e, l=l: e.tensor_tensor(out=ptmp[:, 3, l, :], in0=ptmp[:, 4, l, :], in1=rs, op=ALU.mult), [ptmp_b], [ptmp_b])
        P.op(dve, lambda e: e.memset(ptmp[:, 4, 0, :], 0.0), [ptmp_b], [ptmp_b])
        for l in range(1, L):
            P.op(dve, lambda e, l=l: e.tensor_tensor(out=ptmp[:, 4, l, :], in0=ptmp[:, 4, l - 1, :], in1=ptmp[:, 3, l, :], op=ALU.add), [ptmp_b], [ptmp_b])
        P.op(dve, lambda e: e.tensor_scalar(out=dvs(8), in0=ptmp[:, 4], scalar1=-0.5, scalar2=0.5, op0=ALU.mult, op1=ALU.add), [ptmp_b], [dv_b])
        P.op(dve, lambda e: e.tensor_scalar(out=dvs(9), in0=ptmp[:, 4], scalar1=0.5, scalar2=0.5, op0=ALU.mult, op1=ALU.add), [ptmp_b], [dv_b])
        P.op(dve, lambda e: e.tensor_scalar(out=dvs(10), in0=ptmp[:, 4], scalar1=0.5, scalar2=-0.5, op0=ALU.mult, op1=ALU.add), [ptmp_b], [dv_b])

        stream = {"issued": 0, "consumed": 0, "released": 0}
        units = [(b, j, l) for b in range(NSEQ) for j in range(NTILE) for l in range(L)]
        total_groups = len(units) * NG

        def issue_loads(upto):
            while stream["issued"] < min(upto, total_groups):
                k = stream["issued"]
                u, g = divmod(k, NG)
                l = units[u][2]
                flush_casts(l)
                s = k % NSLOT
                P.dma(sp, wsem[s], wring[:, s, 0:GLEN[g]], wb_d[l, g, :, 0:GLEN[g]], [scr_b[l][g]], [wring_b[s]])
                stream["issued"] += 1

        def next_group():
            k = stream["consumed"]
            issue_loads(stream["released"] + NSLOT)
            assert k < stream["issued"], (k, stream)
            stream["consumed"] += 1
            s = k % NSLOT
            return wring[:, s, :].rearrange("p (b k c) -> p b k c", b=4, k=8), wring_b[s], wring[:, s, :]

        def release(n=1):
            stream["released"] += n
            assert stream["released"] <= stream["consumed"]
            issue_loads(stream["released"] + NSLOT)

        def mm_fm(w, wb, blk, rhs_ap, rhs_b, n=T, reserve=False):
            pa, pb = bank(reserve)
            for kc in range(KC):
                P.op(pe, lambda e, kc=kc: e.matmul(pa[:, 0:n], lhsT=w[:, blk, kc, :], rhs=rhs_ap(kc),
                                                   start=(kc == 0), stop=(kc == KC - 1)),
                     [wb, rhs_b[kc]], [pb])
            return pa, pb

        def mm_tm(w, wb, tb, lhs, lhs_b):
            pa, pb = bank()
            for kc in range(KC):
                P.op(pe, lambda e, kc=kc: e.matmul(pa, lhsT=lhs[:, kc, tb * 128:(tb + 1) * 128], rhs=w[:, :, kc, :],
                                                   start=(kc == 0), stop=(kc == KC - 1)),
                     [wb, lhs_b[kc]], [pb])
            return pa, pb

        def rms_rstd(src_ap, src_bufs, ones_idx, nchunks, from_psum=False):
            pa, pb = bank()
            for c in range(nchunks):
                sq, sqb = T16()
                P.op(act, lambda e, c=c, sq=sq: e.activation(out=sq, in_=src_ap(c), func=AF.Square), [src_bufs[c]], [sqb])
                P.op(pe, lambda e, c=c, sq=sq: e.matmul(pa, lhsT=ones[:, ones_idx, :], rhs=sq, start=(c == 0), stop=(c == nchunks - 1)),
                     [sqb, constb], [pb])
            sd, sdb = T32()
            if DEBUG["stage"] == 0.2:
                return sd, sdb
            P.op(act, lambda e: e.activation(out=sd, in_=pa, func=AF.Sqrt, bias=EPS, scale=1.0), [pb], [sdb])
            rstd, rb = T32()
            if DEBUG["stage"] == 0.4:
                return sd, sdb
            P.op(dve, lambda e: e.reciprocal(out=rstd, in_=sd), [sdb], [rb])
            return rstd, rb

        def gate_merge(l, br, ypa, ypb, mpa, mpb, d):
            th, thb = T32()
            P.op(act, lambda e: e.activation(out=th, in_=mpa, func=AF.Tanh, bias=dv[:, 1 + br, l, d:d + 1], scale=0.5), [mpb, dv_b], [thb])
            if DEBUG["stage"] == 1.7:
                return
            if br == DEBUG["branches"].find("abc"[br]) == 0 or "abc"[br] == DEBUG["branches"][0]:
                P.op(dve, lambda e: e.scalar_tensor_tensor(out=merged[:, d, :], in0=th, scalar=1.0, in1=ypa, op0=ALU.add, op1=ALU.mult),
                     [thb, ypb], [merged_b[d]])
            else:
                tm, tmb = T32()
                P.op(dve, lambda e: e.scalar_tensor_tensor(out=tm, in0=th, scalar=1.0, in1=ypa, op0=ALU.add, op1=ALU.mult),
                     [thb, ypb], [tmb])
                P.op(pool, lambda e: e.tensor_tensor(out=merged[:, d, :], in0=merged[:, d, :], in1=tm, op=ALU.add),
                     [tmb, merged_b[d]], [merged_b[d]])

        def proj_and_merge(l, br, yin, yin_b):
            for half in range(2):
                wm, wmb, _ = next_group()
                wp, wpb, _ = next_group()
                for i in range(4):
                    d = half * 4 + i
                    mpa, mpb = mm_fm(wm, wmb, i, lambda kc: xn[:, kc, :], xn_b)
                    ypa, ypb = mm_fm(wp, wpb, i, lambda kc: yin[:, kc, :], yin_b)
                    if "abc"[br] in DEBUG["branches"] and DEBUG["stage"] != 1.6:
                        gate_merge(l, br, ypa, ypb, mpa, mpb, d)
                release(2)

        def unit(l):
            rstd, rb = rms_rstd(lambda c: xres[:, c, :], xres_b, 0, KC)
            if DEBUG["stage"] < 0.8:
                return
            for c in range(KC):
                P.op(dve, lambda e, c=c: e.scalar_tensor_tensor(out=xn[:, c, :], in0=xres[:, c, :], scalar=pv[:, 0, l, c:c + 1], in1=rstd,
                                                                  op0=ALU.mult, op1=ALU.mult),
                     [xres_b[c], rb, pv_b], [xn_b[c]])

            if DEBUG["stage"] < 1:
                return
            nbf, nbf_b = big[0], big_b[0]
            nview = nbf[:].rearrange("p a t -> p (a t)").rearrange("p (tb n) -> p tb n", tb=4)
            wv = [next_group(), next_group()]
            for tb in range(4):
                for half in range(2):
                    dch = 2 * tb + half
                    pa, pb = mm_tm(wv[half][0], wv[half][1], tb, xn, xn_b)
                    P.op(act, lambda e, pa=pa, dch=dch: e.activation(out=merged[:, dch, :], in_=pa, func=AF.Gelu_apprx_tanh),
                         [pb], [merged_b[dch]])
                    P.op(dve, lambda e, half=half, dch=dch: e.bn_stats(out=stat[:, tb, half * 6:(half + 1) * 6], in_=merged[:, dch, :]),
                         [merged_b[dch]], [stat_b[tb]])
                P.op(dve, lambda e: e.bn_aggr(out=stat[:, tb, 12:14], in_=stat[:, tb, 0:12]), [stat_b[tb]], [stat_b[tb]])
            P.op(act, lambda e: e.activation(out=stat[:, :, 14], in_=stat[:, :, 13], func=AF.Sqrt, bias=EPS, scale=1.0), stat_b, stat_b)
            P.op(dve, lambda e: e.reciprocal(out=stat[:, :, 15], in_=stat[:, :, 14]), stat_b, stat_b)
            for tb in range(4):
                P.op(dve, lambda e: e.tensor_scalar(out=nview[:, tb, :], in0=xio_v[:, tb, :], scalar1=stat[:, tb, 12:13], scalar2=stat[:, tb, 15:16],
                                                    op0=ALU.subtract, op1=ALU.mult),
                     [merged_b[2 * tb], merged_b[2 * tb + 1], stat_b[tb]], nbf_b)
            release(2)
            if DEBUG["stage"] in (1.1, 1.2):
                return
            _, auxb, auxflat = next_group()
            wsT = auxflat[:, 0:1024].rearrange("p (g t) -> p g t", g=8)
            P.op(pool, lambda e: e.tensor_tensor(out=wsm[:], in0=wsT, in1=mask_tril.unsqueeze(1).to_broadcast([128, 8, 128]), op=ALU.mult),
                 [auxb, cst_b], [wsm_b])
            release(1)
            if DEBUG["stage"] == 1.3:
                return
            P.dma(sp, m2sem, m2[:].rearrange("p g t -> p (g t)"), bs_d[l:l + 1, :].partition_broadcast(128), [], [m2_b])
            for hf in range(2):
                pa, pb = bank()
                P.op(pe, lambda e, pa=pa, hf=hf: e.matmul(pa, lhsT=ones[:, 3, :], rhs=wsm[:, hf * 4:(hf + 1) * 4, :], start=True, stop=True),
                     [wsm_b, constb], [pb])
                for gg in range(4):
                    g = hf * 4 + gg
                    P.op(dve, lambda e, pa=pa, gg=gg, g=g: e.scalar_tensor_tensor(out=m2[:, g, :], in0=pa[:, gg * 128:(gg + 1) * 128], scalar=pv[:, 6, l, g:g + 1],
                                                                                  in1=m2[:, g, :], op0=ALU.mult, op1=ALU.add),
                         [pb, m2_b, pv_b], [m2_b])
            if DEBUG["stage"] == 1.4:
                return
            yin, yin_b = big[1], big_b[1]
            for half in range(2):
                wu, wub, _ = next_group()
                wg, wgb, _ = next_group()
                for pair in range(2):
                    ii = [pair * 2, pair * 2 + 1]
                    st_ = {}
                    for i in ii:
                        upa, upb = mm_fm(wu, wub, i, lambda kc: xn[:, kc, :], xn_b)
                        ug, ugb = T16()
                        P.op(act, lambda e: e.activation(out=ug, in_=upa, func=AF.Gelu_apprx_tanh), [upb], [ugb])
                        st_[i] = {"ug": (ug, ugb)}
                    for i in ii:
                        st_[i]["g"] = mm_fm(wg, wgb, i, lambda kc: xn[:, kc, :], xn_b)
                    for i in ii:
                        c = half * 4 + i
                        spa, spb = bank()
                        for tb in range(4):
                            P.op(pe, lambda e, tb=tb: e.matmul(spa[:, tb * 128:(tb + 1) * 128], lhsT=nview[:, tb, c * 128:(c + 1) * 128], rhs=wsm[:, c, :],
                                                               start=True, stop=True),
                                 nbf_b + [wsm_b], [spb])
                        st_[i]["sp"] = (spa, spb)
                    for i in ii:
                        gpa, gpb = st_[i]["g"]
                        sg, sgb = T16()
                        P.op(act, lambda e: e.activation(out=sg, in_=gpa, func=AF.Silu), [gpb], [sgb])
                        st_[i]["sg"] = (sg, sgb)
                    for i in ii:
                        c = half * 4 + i
                        spa, spb = st_[i]["sp"]
                        ug, ugb = st_[i]["ug"]
                        sg, sgb = st_[i]["sg"]
                        mx_, mxb = T32()
                        P.op(dve, lambda e: e.scalar_tensor_tensor(out=mx_.rearrange("p (a t) -> p a t", a=4), in0=spa.rearrange("p (a t) -> p a t", a=4),
                                                                   scalar=pv[:, 5, l, c:c + 1],
                                                                   in1=m2[:, c, :].unsqueeze(1).to_broadcast([128, 4, 128]), op0=ALU.mult, op1=ALU.add),
                             [spb, m2_b, pv_b], [mxb])
                        P.op(pool, lambda e: e.tensor_tensor(out=ug, in0=ug, in1=sg, op=ALU.mult), [ugb, sgb], [ugb])
                        P.op(pool, lambda e: e.tensor_tensor(out=yin[:, c, :], in0=mx_, in1=ug, op=ALU.mult), [mxb, ugb], [yin_b[c]])
                release(2)
            if DEBUG["stage"] == 1.5:
                return
            proj_and_merge(l, 0, yin, yin_b)

            if DEBUG["stage"] < 2:
                return
            _, auxb, auxflat = next_group()
            P.op(pool, lambda e: e.tensor_copy(out=wri[:], in_=auxflat[:, 0:2048]), [auxb], [wri_b])
            release(1)
            auxb = wri_b
            wr = wri[:, 0:1024].rearrange("p (h j) -> p h j", h=8)
            wi = wri[:, 1024:2048].rearrange("p (h j) -> p h j", h=8)
            P.op(pool, lambda e: e.tensor_copy(out=xl[:, :, 0:3], in_=xtail[:, l]), [xtail_b[l]], xl_b)
            yin, yin_b = big[2], big_b[2]
            for half in range(2):
                wx, wxb, _ = next_group()
                wg, wgb, _ = next_group()
                for k in range(4):
                    P.op(pool, lambda e, k=k: e.tensor_tensor(out=dg[:, k], in0=ident.unsqueeze(1).to_broadcast([128, 4, 128]),
                                                               in1=pv[:, 7 + k, l, half * 4:half * 4 + 4].unsqueeze(2).to_broadcast([128, 4, 128]), op=ALU.mult),
                         [cst_b, pv_b], [dg_b])
                for pair in range(2):
                    ii = [pair * 2, pair * 2 + 1]
                    cc_ = [half * 4 + i for i in ii]
                    st_ = {}
                    for i, c in zip(ii, cc_):
                        xpa, xpb = mm_fm(wx, wxb, i, lambda kc: xn[:, kc, :], xn_b)
                        P.op(dve, lambda e: e.tensor_copy(out=xl[:, c, 3:3 + T], in_=xpa), [xpb], [xl_b[c]])
                    for i, c in zip(ii, cc_):
                        cpa, cpb = bank()
                        for k in range(4):
                            P.op(pe, lambda e, k=k: e.matmul(cpa, lhsT=dg[:, k, i, :], rhs=xl[:, c, k:k + T], start=(k == 0), stop=(k == 3)),
                                 [dg_b, xl_b[c]], [cpb])
                        xbf, xbfb = T32()
                        P.op(act, lambda e: e.activation(out=xbf, in_=cpa, func=AF.Identity, bias=pv[:, 11, l, c:c + 1], scale=1.0), [cpb, pv_b], [xbfb])
                        xbb, xbbb = T16()
                        P.op(dve, lambda e: e.tensor_scalar(out=xbb, in0=cpa, scalar1=pv[:, 11, l, c:c + 1], scalar2=None, op0=ALU.add), [cpb, pv_b], [xbbb])
                        st_[c] = {"xbf": (xbf, xbfb), "xbb": (xbb, xbbb)}
                    for i, c in zip(ii, cc_):
                        xbb, xbbb = st_[c]["xbb"]
                        rpa, rpb = bank()
                        P.op(pe, lambda e: e.matmul(rpa, lhsT=wr[:, c, :], rhs=xbb, start=True, stop=True), [auxb, xbbb], [rpb])
                        ipa, ipb = bank()
                        P.op(pe, lambda e: e.matmul(ipa, lhsT=wi[:, c, :], rhs=xbb, start=True, stop=True), [auxb, xbbb], [ipb])
                        st_[c]["r"] = (rpa, rpb)
                        st_[c]["i"] = (ipa, ipb)
                    for i, c in zip(ii, cc_):
                        rpa, rpb = st_[c]["r"]
                        ipa, ipb = st_[c]["i"]
                        thr, thrb = T32()
                        P.op(act, lambda e: e.activation(out=thr, in_=rpa, func=AF.Tanh, bias=dv[:, 4, l, c:c + 1], scale=0.5), [rpb, dv_b], [thrb])
                        thi, thib = T32()
                        P.op(act, lambda e: e.activation(out=thi, in_=ipa, func=AF.Tanh, bias=dv[:, 5, l, c:c + 1], scale=0.5), [ipb, dv_b], [thib])
                        st_[c]["thr"] = (thr, thrb)
                        st_[c]["thi"] = (thi, thib)
                    for i, c in zip(ii, cc_):
                        st_[c]["g"] = mm_fm(wg, wgb, i, lambda kc: xn[:, kc, :], xn_b)
                    for i, c in zip(ii, cc_):
                        thr, thrb = st_[c]["thr"]
                        a2, a2b = T32()
                        P.op(act, lambda e: e.activation(out=thr, in_=thr, func=AF.Exp, bias=dv[:, 6, l, c:c + 1], scale=dv[:, 6, l, c:c + 1]), [thrb, dv_b], [thrb])
                        P.op(pool, lambda e: e.tensor_tensor(out=a2, in0=thr, in1=thr, op=ALU.mult), [thrb], [a2b])
                        st_[c]["a2"] = (a2, a2b)
                    for i, c in zip(ii, cc_):
                        a2, a2b = st_[c]["a2"]
                        P.op(act, lambda e: e.activation(out=a2, in_=a2, func=AF.Sqrt, bias=0.25, scale=-0.25), [a2b], [a2b])
                    for i, c in zip(ii, cc_):
                        gpa, gpb = st_[c]["g"]
                        sg, sgb = T16()
                        P.op(act, lambda e: e.activation(out=sg, in_=gpa, func=AF.Silu), [gpb], [sgb])
                        st_[c]["sg"] = (sg, sgb)
                    for i, c in zip(ii, cc_):
                        xbf, xbfb = st_[c]["xbf"]
                        thi, thib = st_[c]["thi"]
                        aa, aab = st_[c]["thr"]
                        a2, a2b = st_[c]["a2"]
                        sg, sgb = st_[c]["sg"]
                        P.op(dve, lambda e: e.scalar_tensor_tensor(out=thi, in0=thi, scalar=1.0, in1=xbf, op0=ALU.add, op1=ALU.mult), [thib, xbfb], [thib])
                        P.op(pool, lambda e: e.tensor_tensor(out=thi, in0=thi, in1=a2, op=ALU.mult), [thib, a2b], [thib])
                        P.op(dve, lambda e: e.tensor_tensor_scan(out=xbf, data0=aa, data1=thi, initial=lruh[:, l, c:c + 1], op0=ALU.mult, op1=ALU.add),
                             [aab, thib, lruh_b[l][c]], [xbfb])
                        P.op(pool, lambda e: e.tensor_copy(out=lruh[:, l, c:c + 1], in_=xbf[:, T - 1:T]), [xbfb], [lruh_b[l][c]])
                        P.op(pool, lambda e: e.tensor_tensor(out=yin[:, c, :], in0=xbf, in1=sg, op=ALU.mult), [xbfb, sgb], [yin_b[c]])
                release(2)
            P.op(pool, lambda e: e.tensor_copy(out=xtail[:, l], in_=xl[:, :, T:T + 3]), xl_b, [xtail_b[l]])
            proj_and_merge(l, 1, yin, yin_b)

            if DEBUG["stage"] < 3:
                return
            vt_, vt_b = big[0], big_b[0]
            vview = vt_[:].rearrange("p a t -> p (a t)").rearrange("p (tb n) -> p tb n", tb=4)
            wv = [next_group(), next_group()]
            for tb in range(4):
                for half in range(2):
                    pa, pb = mm_tm(wv[half][0], wv[half][1], tb, xn, xn_b)
                    P.op(act, lambda e, pa=pa, half=half, tb=tb: e.activation(out=vview[:, tb, half * 512:(half + 1) * 512], in_=pa, func=AF.Copy),
                         [pb], vt_b)
            release(2)
            yin, yin_b = big[1], big_b[1]
            qg_, qg_b = big[2], big_b[2]
            kd_, kd_b = big[3], big_b[3]
            for hg in range(2):
                wq, wqb, _ = next_group()
                wf, wfb, _ = next_group()
                for pair in range(2):
                    ii = [pair * 2, pair * 2 + 1]
                    st_ = {}
                    for i in ii:
                        fpa, fpb = mm_fm(wf, wfb, i, lambda kc: xn[:, kc, :], xn_b)
                        th, thb = T32()
                        P.op(act, lambda e: e.activation(out=th, in_=fpa, func=AF.Tanh, scale=0.5), [fpb], [thb])
                        st_[i] = {"th": (th, thb)}
                    for i in ii:
                        st_[i]["q"] = mm_fm(wq, wqb, i, lambda kc: xn[:, kc, :], xn_b)
                    for i in ii:
                        h = hg * 4 + i
                        th, thb = st_[i]["th"]
                        ff, ffb = T32()
                        P.op(dve, lambda e: e.tensor_scalar(out=ff, in0=th, scalar1=dv[:, 8, l, h:h + 1], scalar2=dv[:, 9, l, h:h + 1], op0=ALU.mult, op1=ALU.add),
                             [thb, dv_b], [ffb])
                        G, Gb = T32()
                        P.op(dve, lambda e: e.tensor_tensor_scan(out=G, data0=rmask, data1=ff, initial=1.0, op0=ALU.max, op1=ALU.mult), [ffb, cst_b], [Gb])
                        P.op(pool, lambda e: e.tensor_copy(out=glast[:, i, :], in_=G.rearrange("p (c t) -> p c t", t=32)[:, :, 31]), [Gb], [glast_b[i]])
                        rG = ff
                        P.op(dve, lambda e: e.reciprocal(out=rG, in_=G), [Gb, ffb], [ffb])
                        P.op(pool, lambda e: e.tensor_scalar(out=th, in0=th, scalar1=dv[:, 10, l, h:h + 1], scalar2=dv[:, 8, l, h:h + 1], op0=ALU.mult, op1=ALU.add),
                             [thb, dv_b], [thb])
                        kg, kgb = T16()
                        P.op(pool, lambda e: e.tensor_tensor(out=kg, in0=th, in1=rG, op=ALU.mult), [thb, ffb], [kgb])
                        kdec, kdecb = T16()
                        P.op(pool, lambda e: e.tensor_tensor(out=kdec.rearrange("p (c t) -> p c t", t=32), in0=kg.rearrange("p (c t) -> p c t", t=32),
                                                             in1=glast[:, i, :].unsqueeze(2).to_broadcast([128, 16, 32]), op=ALU.mult),
                             [kgb, glast_b[i]], [kdecb])
                        qpa, qpb = st_[i]["q"]
                        P.op(dve, lambda e: e.tensor_tensor(out=qg_[:, i, :], in0=qpa, in1=G, op=ALU.mult), [qpb, Gb], [qg_b[i]])
                        st_[i]["kg"] = (kg, kgb)
                        st_[i]["kdec"] = (kdec, kdecb)
                    for i in ii:
                        kg, kgb = st_[i]["kg"]
                        kdec, kdecb = st_[i]["kdec"]
                        spa, spb = bank()
                        for tb in range(4):
                            P.op(pe, lambda e, tb=tb: e.matmul(spa[:, tb * 128:(tb + 1) * 128], lhsT=kg[:, tb * 128:(tb + 1) * 128],
                                                               rhs=qg_[:, i, tb * 128:(tb + 1) * 128], start=True, stop=True),
                                 [kgb, qg_b[i]], [spb])
                        P.op(dve, lambda e: e.tensor_tensor(out=kd_[:, 4 + i, :], in0=spa, in1=mask_ch, op=ALU.mult), [spb, cst_b], [kd_b[4 + i]])
                        tpa, tpb = bank()
                        tpbf = tpa.bitcast(BF16)
                        for tb in range(4):
                            P.op(pe, lambda e, tb=tb: e.transpose(tpbf[:, tb * 128:(tb + 1) * 128], kdec[:, tb * 128:(tb + 1) * 128], identb[:]),
                                 [kdecb, constb], [tpb])
                        P.op(act, lambda e: e.activation(out=kd_[:, i, :], in_=tpbf[:, 0:512], func=AF.Copy), [tpb], [kd_b[i]])
                release(2)
                wgc, wgcb, _ = next_group()
                for i in range(4):
                    h = hg * 4 + i
                    P.op(act, lambda e, i=i, h=h: e.activation(out=sall[:, 0, i * 128:(i + 1) * 128], in_=hgst[:, l, h * 128:(h + 1) * 128], func=AF.Copy),
                         [hgst_b[l][h]], [sall_b[0]])
                sgs = {}
                for c in range(16):
                    tb, cc = divmod(c, 4)
                    upa, upb = bank()
                    for i in range(4):
                        h = hg * 4 + i
                        P.op(pe, lambda e, i=i, h=h: e.matmul(upa[:, i * 128:(i + 1) * 128], lhsT=kd_[cc * 32:(cc + 1) * 32, i, tb * 128:(tb + 1) * 128],
                                                             rhs=vview[cc * 32:(cc + 1) * 32, tb, h * 128:(h + 1) * 128], start=True, stop=True,
                                                             tile_position=(cc * 32, 0)),
                             [kd_b[i]] + vt_b, [upb])
                    for i in range(4):
                        h = hg * 4 + i
                        if c == 0:
                            src, srcb = hgst[:, l, h * 128:(h + 1) * 128], hgst_b[l][h]
                        else:
                            src, srcb = stw[:, c % 2, i * 128:(i + 1) * 128], stw_b[c % 2][i]
                        if c == 15:
                            dst, dstb = hgst[:, l, h * 128:(h + 1) * 128], hgst_b[l][h]
                        else:
                            dst, dstb = stw[:, (c + 1) % 2, i * 128:(i + 1) * 128], stw_b[(c + 1) % 2][i]
                        P.op(dve, lambda e, i=i, src=src, dst=dst: e.scalar_tensor_tensor(out=dst, in0=src, scalar=glast[:, i, c:c + 1], in1=upa[:, i * 128:(i + 1) * 128],
                                                                                         op0=ALU.mult, op1=ALU.add),
                             [srcb, glast_b[i], upb], [dstb])
                        if c < 15:
                            P.op(act, lambda e, i=i, dst=dst: e.activation(out=sall[:, c + 1, i * 128:(i + 1) * 128], in_=dst, func=AF.Copy),
                                 [dstb], [sall_b[c + 1]])
                    if c % 4 == 3:
                        i = c // 4
                        gpa, gpb = mm_fm(wgc, wgcb, i, lambda kc: xn[:, kc, :], xn_b)
                        P.op(act, lambda e, i=i, gpa=gpa: e.activation(out=sgc[:, i, :], in_=gpa, func=AF.Silu), [gpb], [sgc_b[i]])
                release(1)
                for i in range(4):
                    h = hg * 4 + i
                    opa, opb = bank()
                    for tb in range(4):
                        P.op(pe, lambda e, tb=tb, h=h, i=i: e.matmul(opa[:, tb * 128:(tb + 1) * 128], lhsT=vview[:, tb, h * 128:(h + 1) * 128],
                                                                     rhs=kd_[:, 4 + i, tb * 128:(tb + 1) * 128], start=True, stop=False),
                             vt_b + [kd_b[4 + i]], [opb])
                        for cc in range(4):
                            c = tb * 4 + cc
                            P.op(pe, lambda e, c=c, i=i, cc=cc: e.matmul(opa[:, c * 32:(c + 1) * 32], lhsT=sall[:, c, i * 128:(i + 1) * 128],
                                                                         rhs=qg_[:, i, c * 32:(c + 1) * 32], start=False, stop=(cc == 3)),
                                 [sall_b[c], qg_b[i]], [opb])
                    rstd, rb = rms_rstd(lambda c_: opa, [opb], 2, 1)
                    on, onb = T32()
                    P.op(dve, lambda e: e.scalar_tensor_tensor(out=on, in0=opa, scalar=pv[:, 16, l, h:h + 1], in1=rstd, op0=ALU.mult, op1=ALU.mult),
                         [opb, rb, pv_b], [onb])
                    P.op(pool, lambda e: e.tensor_tensor(out=yin[:, h, :], in0=on, in1=sgc[:, i, :], op=ALU.mult), [onb, sgc_b[i]], [yin_b[h]])
            proj_and_merge(l, 2, yin, yin_b)

            if DEBUG["stage"] < 4:
                return
            mbf, mbf_b = big[2], big_b[2]
            for c in range(KC):
                P.op(act, lambda e, c=c: e.activation(out=mbf[:, c, :], in_=merged[:, c, :], func=AF.Copy), [merged_b[c]], [mbf_b[c]])
            ppa, ppb = bank(reserve=True)
            pend = []

            def flush_pend():
                while pend:
                    d_, sq_, sqb_ = pend.pop(0)
                    P.op(pe, lambda e: e.matmul(ppa, lhsT=ones[:, 1, :], rhs=sq_, start=(d_ == 0), stop=(d_ == 7)), [sqb_, constb], [ppb])
            for half in range(2):
                wo, wob, _ = next_group()
                for i in range(4):
                    d = half * 4 + i
                    ypa, ypb = mm_fm(wo, wob, i, lambda kc: mbf[:, kc, :], mbf_b)
                    flush_pend()
                    sq, sqb = T16()
                    P.op(act, lambda e: e.activation(out=sq, in_=ypa, func=AF.Square), [ypb], [sqb])
                    pend.append((d, sq, sqb))
                    P.op(dve, lambda e, d=d: e.tensor_scalar(out=merged[:, d, :], in0=ypa, scalar1=dv[:, 0, l, d:d + 1], scalar2=None, op0=ALU.mult),
                         [ypb, dv_b], [merged_b[d]])
                release(1)
            flush_pend()
            resv.clear()
            sd, sdb = T32()
            P.op(act, lambda e: e.activation(out=sd, in_=ppa, func=AF.Sqrt, bias=EPS, scale=1.0), [ppb], [sdb])
            rstd, rb = T32()
            P.op(dve, lambda e: e.reciprocal(out=rstd, in_=sd), [sdb], [rb])
            for d in range(KC):
                P.op(dve, lambda e, d=d: e.tensor_tensor(out=merged[:, d, :], in0=merged[:, d, :], in1=rstd, op=ALU.mult), [merged_b[d], rb], [merged_b[d]])
                P.op(pool, lambda e, d=d: e.tensor_tensor(out=xres[:, d, :], in0=xres[:, d, :], in1=merged[:, d, :], op=ALU.add),
                     [merged_b[d], xres_b[d]], [xres_b[d]])

        xio = merged[:].rearrange("p a t -> p (a t)").rearrange("p (tb n) -> p tb n", tb=4)
        for b in range(NSEQ):
            if b > 0:
                P.op(dve, lambda e: e.memset(hgst[:].rearrange("p l n -> p (l n)"), 0.0), [], [bb for bl in hgst_b for bb in bl])
                P.op(dve, lambda e: e.memset(lruh[:].rearrange("p l n -> p (l n)"), 0.0), [], [bb for bl in lruh_b for bb in bl])
                P.op(dve, lambda e: e.memset(xtail[:].rearrange("p l c k -> p (l c k)"), 0.0), [], xtail_b)
            for j in range(NTILE):
                P.dma(sp, iosem, xio, x_d[b, j * T:(j + 1) * T, :].rearrange("(tb p) n -> p tb n", p=128), [], merged_b)
                for c in range(KC):
                    pa, pb = bank()
                    for tb in range(4):
                        P.op(pe, lambda e, tb=tb, c=c: e.transpose(pa[:, tb * 128:(tb + 1) * 128], xio[:, tb, c * 128:(c + 1) * 128], ident),
                             merged_b + [cst_b], [pb])
                    P.op(act, lambda e, c=c, pa=pa: e.activation(out=xres[:, c, :], in_=pa, func=AF.Copy), [pb], [xres_b[c]])
                for l in range(L):
                    if b == 0 and j == 0:
                        hook_state["layer"] = l + 1
                        hook_state["n"] = 0
                        P.pool_hook = pool_hook
                    unit(l)
                    P.pool_hook = None
                for tb in range(4):
                    for hf in range(2):
                        pa, pb = bank()
                        for cc in range(4):
                            c = hf * 4 + cc
                            P.op(pe, lambda e, cc=cc, c=c, tb=tb: e.transpose(pa[:, cc * 128:(cc + 1) * 128], xres[:, c, tb * 128:(tb + 1) * 128], ident),
                                 [xres_b[c], cst_b], [pb])
                        P.op(act, lambda e, pa=pa, tb=tb, hf=hf: e.activation(out=xio[:, tb, hf * 512:(hf + 1) * 512], in_=pa, func=AF.Copy), [pb], merged_b)
                P.dma(sp, iosem, out_d[b, j * T:(j + 1) * T, :].rearrange("(tb p) n -> p tb n", p=128), xio, merged_b, [])
        if P.dry:
            return P.waited
        nc.gpsimd.wait_ge(iosem[0], iosem[1])
        stats = {e.name: (e.n, e.ninc) for e in (pe, act, dve, pool)}
        print("instr counts (ops, sem incs)", stats, "groups", stream)
    return nc


def _colblock(w, b0):
    sub = w[:, b0 * 128:(b0 + 4) * 128]
    return sub.reshape(8, 128, 4, 128).transpose(1, 2, 0, 3)


def make_consts():
    cst = np.zeros((128, 1280), np.float32)
    cst[:, 0:128] = np.eye(128, dtype=np.float32)
    s = np.arange(128)[:, None]
    t = np.arange(128)[None, :]
    cst[:, 128:256] = (s <= t).astype(np.float32)
    mch = ((s // 32 == t // 32) & (s <= t)).astype(np.float32)
    cst[:, 256:768] = np.tile(mch, (1, 4))
    rm = np.zeros(512, np.float32)
    rm[::32] = 1.0
    cst[:, 768:1280] = rm[None, :]
    return cst


def prep_shared(inp, L):
    ws = np.empty((L, NG, 128, GSZ), np.float32)
    proj = {"a": inp["w_a_proj"], "b": inp["w_b_proj"], "c": inp["w_c_proj"], "o": inp["w_out"]}
    for l in range(L):
        for g, (kind, b0) in enumerate(GROUPS):
            if kind == "in":
                ws[l, g] = _colblock(inp["w_in"][l], b0).reshape(128, GSZ)
            elif kind == "auxA":
                ws[l, g] = 0.0
                ws[l, g, :, 0:1024] = inp["gm_w_s"][l].transpose(2, 0, 1).reshape(128, 1024)
            elif kind == "auxB":
                ws[l, g] = 0.0
                ws[l, g, :, 0:1024] = inp["lru_w_r"][l].transpose(1, 0, 2).reshape(128, 1024)
                ws[l, g, :, 1024:2048] = inp["lru_w_i"][l].transpose(1, 0, 2).reshape(128, 1024)
            else:
                ws[l, g] = _colblock(proj[kind][l], b0).reshape(128, GSZ)
    names = [("pre_norm_g", None), ("post_norm_g", None), ("b_merge", 0), ("b_merge", 1), ("b_merge", 2),
             ("gm_ln_g", None), ("gm_ln_b", None), ("lru_conv_w", 0), ("lru_conv_w", 1), ("lru_conv_w", 2), ("lru_conv_w", 3),
             ("lru_conv_b", None), ("lru_b_r", None), ("lru_b_i", None), ("lru_lambda", None), ("hg_lower_bounds", None), ("hg_norm_g", None)]
    pvec = np.empty((128, NPV, L, 8), np.float32)
    for i, (n, k) in enumerate(names):
        a = inp[n][:L] if k is None else inp[n][:L, k]
        pvec[:, i] = a.reshape(L, 8, 128).transpose(2, 0, 1)
    bsrow = np.ascontiguousarray(inp["gm_b_s"][:L].reshape(L, 1024))
    return {"wstream": ws, "pvec": pvec.reshape(128, -1), "bsrow": bsrow, "cst": make_consts()}


def run(inputs, L, n_cores, nseq_per_core, ntile):
    inp = {k: np.asarray(v) for k, v in inputs.items()}
    shared = prep_shared(inp, L)
    need = build_program(L, nseq_per_core, ntile, None)
    nc = build_program(L, nseq_per_core, ntile, need)
    x = np.ascontiguousarray(inp["x"], dtype=np.float32)
    in_maps = []
    for c in range(n_cores):
        m = dict(shared)
        m["x"] = np.ascontiguousarray(x[c * nseq_per_core:(c + 1) * nseq_per_core])
        in_maps.append(m)
    res = run_bass_kernel_spmd(nc, in_maps, core_ids=list(range(n_cores)))
    return np.concatenate([r["out"] for r in res.results], axis=0)


def kernel(**inputs):
    return run(inputs, 4, 8, 4, 4)
```
